# Optimizing a Trainium2 kernel written in Bass

```python
import math
import jax
import jax.numpy as jnp
from jax import lax
import numpy as np

D_MODEL = 1024
BATCH = 4
SEQ = 4096
DEPTH = 1

CHUNK = 64
QBLOCK = 128
SB_HEADS = 8
HEAD_DIM = 64
SB_WIDTH = SB_HEADS * HEAD_DIM
POOL_WINDOWS = (2, 4, 8, 16)
N_POOL_GROUPS = len(POOL_WINDOWS)
POOL_WIDTH = 512
POOL_GROUP_DIM = POOL_WIDTH // N_POOL_GROUPS
MAX_WIN = max(POOL_WINDOWS)
IN_WIDTH = 3 * SB_WIDTH + POOL_WIDTH + 2 * D_MODEL
N_EXPERTS = 64
EXPERT_DIM = 128
TOP_K = 8
N_EXPERT_GROUPS = 8
TOPK_GROUPS = 4
SHARED_DIM = 256
ROUTED_SCALE = 2.5
NORM_EPS = 1e-6

kernel_name = "hybrid_stickbreak_pool_moe_block"


def _rmsnorm(x, g):
    xf = x.astype(jnp.float32)
    r = lax.rsqrt(jnp.mean(xf * xf, axis=-1, keepdims=True) + NORM_EPS)
    return (xf * r).astype(x.dtype) * g


def _stick_breaking(q, k, v):
    S = q.shape[2]
    scale = 1.0 / math.sqrt(HEAD_DIM)
    outs = []
    for i0 in range(0, S, QBLOCK):
        kn = i0 + QBLOCK
        qb = q[:, :, i0:kn]
        kb = k[:, :, :kn]
        vb = v[:, :, :kn]
        z = jnp.einsum('bhqd,bhkd->bhqk', qb, kb).astype(jnp.float32) * scale
        qpos = i0 + jnp.arange(QBLOCK)[:, None]
        kpos = jnp.arange(kn)[None, :]
        mask = kpos < qpos
        log_fail = jnp.where(mask, jax.nn.log_sigmoid(-z), 0.0)
        between = lax.cumsum(log_fail, axis=3, reverse=True) - log_fail
        a = jnp.where(mask, jnp.exp(jax.nn.log_sigmoid(z) + between), 0.0)
        outs.append(jnp.einsum('bhqk,bhkd->bhqd', a.astype(vb.dtype), vb))
    return jnp.concatenate(outs, axis=2)


def _multiscale_pool(u, w_pool_group, pool_scale):
    B, S, _ = u.shape
    uf = u.astype(jnp.float32).reshape(B, S, N_POOL_GROUPS, POOL_GROUP_DIM)
    cs = jnp.pad(jnp.cumsum(uf, axis=1), ((0, 0), (MAX_WIN, 0), (0, 0), (0, 0)))
    cur = cs[:, MAX_WIN:]
    prev = jnp.stack([cs[:, MAX_WIN - w:MAX_WIN - w + S, gi]
                      for gi, w in enumerate(POOL_WINDOWS)], axis=2)
    count = jnp.minimum(jnp.arange(S)[:, None] + 1,
                        jnp.array(POOL_WINDOWS)[None, :]).astype(jnp.float32)
    mixed = (cur - prev) / count[None, :, :, None] - uf
    mixed = jnp.einsum('bsgc,gcd->bsgd', mixed.astype(u.dtype), w_pool_group)
    return mixed.reshape(B, S, POOL_WIDTH) * pool_scale


def _moe(h, w_router, router_bias, w_exp_gate, w_exp_up, w_exp_down,
         w_sh_gate, w_sh_up, w_sh_down):
    T = h.shape[0]
    per_group = N_EXPERTS // N_EXPERT_GROUPS
    scores = jax.nn.sigmoid((h @ w_router).astype(jnp.float32))
    sel = scores + router_bias.astype(jnp.float32)
    grp_score = jnp.sum(lax.top_k(sel.reshape(T, N_EXPERT_GROUPS, per_group), 2)[0], axis=-1)
    _, gidx = lax.top_k(grp_score, TOPK_GROUPS)
    gmask = jnp.any(gidx[:, :, None] == jnp.arange(N_EXPERT_GROUPS)[None, None, :], axis=1)
    emask = jnp.repeat(gmask, per_group, axis=1)
    _, eidx = lax.top_k(jnp.where(emask, sel, -jnp.inf), TOP_K)
    w = jnp.take_along_axis(scores, eidx, axis=1)
    w = w / jnp.sum(w, axis=-1, keepdims=True) * ROUTED_SCALE
    gates = jnp.zeros((T, N_EXPERTS), jnp.float32).at[jnp.arange(T)[:, None], eidx].set(w)
    a = jnp.einsum('td,edf->tef', h, w_exp_gate)
    b = jnp.einsum('td,edf->tef', h, w_exp_up)
    hmid = (jax.nn.silu(a) * b * gates[:, :, None]).astype(h.dtype)
    routed = jnp.einsum('tef,efd->td', hmid, w_exp_down)
    shared = (jax.nn.silu(h @ w_sh_gate) * (h @ w_sh_up)) @ w_sh_down
    return routed + shared


def _layer(x, c, w_ada, b_ada, g_norm1, w_in, g_q, g_k, w_sb_out, w_pool_group,
           pool_scale, w_pool_out, w_out, g_norm2, w_router, router_bias,
           w_exp_gate, w_exp_up, w_exp_down, w_sh_gate, w_sh_up, w_sh_down):
    B, S, D = x.shape
    mod = (jax.nn.silu(c) @ w_ada + b_ada)[:, None, :]
    shift1, scale1, gate1, shift2, scale2, gate2 = jnp.split(mod, 6, axis=-1)

    h = _rmsnorm(x, g_norm1) * (1 + scale1) + shift1
    proj = h @ w_in
    cuts = [SB_WIDTH, 2 * SB_WIDTH, 3 * SB_WIDTH, 3 * SB_WIDTH + POOL_WIDTH,
            3 * SB_WIDTH + POOL_WIDTH + D_MODEL]
    q, k, v, u, g_a, g_b = jnp.split(proj, cuts, axis=-1)

    def heads(t):
        return t.reshape(B, S, SB_HEADS, HEAD_DIM).transpose(0, 2, 1, 3)

    q = _rmsnorm(heads(q), g_q)
    k = _rmsnorm(heads(k), g_k)
    o_sb = _stick_breaking(q, k, heads(v)).transpose(0, 2, 1, 3).reshape(B, S, SB_WIDTH)
    y_sb = o_sb @ w_sb_out
    y_pool = _multiscale_pool(u, w_pool_group, pool_scale) @ w_pool_out
    merged = jax.nn.sigmoid(g_a) * y_sb + jax.nn.sigmoid(g_b) * y_pool
    x = x + gate1 * (merged @ w_out)

    h2 = _rmsnorm(x, g_norm2) * (1 + scale2) + shift2
    y = _moe(h2.reshape(B * S, D), w_router, router_bias, w_exp_gate, w_exp_up,
             w_exp_down, w_sh_gate, w_sh_up, w_sh_down).reshape(B, S, D)
    return x + gate2 * y


def setup_inputs(seed: int = 0) -> dict:
    key = jax.random.key(seed)
    ks = jax.random.split(key, 24)
    D, L = D_MODEL, DEPTH

    def nrm(k, shape, fan_in, mult=1.0):
        return jax.random.normal(k, shape, jnp.float32) * (mult * fan_in ** -0.5)

    def gain(k, shape):
        return 1.0 + 0.02 * jax.random.normal(k, shape, jnp.float32)

    return {
        "x": jax.random.normal(ks[0], (BATCH, SEQ, D), jnp.float32),
        "c": jax.random.normal(ks[1], (BATCH, D), jnp.float32),
        "w_ada": nrm(ks[2], (L, D, 6 * D), D, 0.5),
        "b_ada": 0.02 * jax.random.normal(ks[3], (L, 6 * D), jnp.float32),
        "g_norm1": gain(ks[4], (L, D)),
        "w_in": nrm(ks[5], (L, D, IN_WIDTH), D),
        "g_q": gain(ks[6], (L, HEAD_DIM)),
        "g_k": gain(ks[7], (L, HEAD_DIM)),
        "w_sb_out": nrm(ks[8], (L, SB_WIDTH, D), SB_WIDTH),
        "w_pool_group": nrm(ks[9], (L, N_POOL_GROUPS, POOL_GROUP_DIM, POOL_GROUP_DIM), POOL_GROUP_DIM),
        "pool_scale": gain(ks[10], (L, POOL_WIDTH)),
        "w_pool_out": nrm(ks[11], (L, POOL_WIDTH, D), POOL_WIDTH),
        "w_out": nrm(ks[12], (L, D, D), D),
        "g_norm2": gain(ks[13], (L, D)),
        "w_router": nrm(ks[14], (L, D, N_EXPERTS), D),
        "router_bias": 0.01 * jax.random.normal(ks[15], (L, N_EXPERTS), jnp.float32),
        "w_exp_gate": nrm(ks[16], (L, N_EXPERTS, D, EXPERT_DIM), D),
        "w_exp_up": nrm(ks[17], (L, N_EXPERTS, D, EXPERT_DIM), D),
        "w_exp_down": nrm(ks[18], (L, N_EXPERTS, EXPERT_DIM, D), EXPERT_DIM),
        "w_sh_gate": nrm(ks[19], (L, D, SHARED_DIM), D),
        "w_sh_up": nrm(ks[20], (L, D, SHARED_DIM), D),
        "w_sh_down": nrm(ks[21], (L, SHARED_DIM, D), SHARED_DIM),
    }


def reference(x, c, w_ada, b_ada, g_norm1, w_in, g_q, g_k, w_sb_out, w_pool_group,
              pool_scale, w_pool_out, w_out, g_norm2, w_router, router_bias,
              w_exp_gate, w_exp_up, w_exp_down, w_sh_gate, w_sh_up, w_sh_down):
    for l in range(DEPTH):
        x = _layer(x, c, w_ada[l], b_ada[l], g_norm1[l], w_in[l], g_q[l], g_k[l],
                   w_sb_out[l], w_pool_group[l], pool_scale[l], w_pool_out[l],
                   w_out[l], g_norm2[l], w_router[l], router_bias[l],
                   w_exp_gate[l], w_exp_up[l], w_exp_down[l],
                   w_sh_gate[l], w_sh_up[l], w_sh_down[l])
    return x
```

```python
import numpy as np
from contextlib import ExitStack

import concourse.bass as bass
import concourse.mybir as mybir
from concourse.bass_utils import run_bass_kernel_spmd

F32 = mybir.dt.float32
BF16 = mybir.dt.bfloat16
AF = mybir.ActivationFunctionType
ALU = mybir.AluOpType

NCORES = 8
S = 4096
D = 1024
NT_OWN = 4
TOK_OWN = 2048
EPS = 1e-6
NEG = -30000.0
ARENA_BYTES = 206 * 1024
NDUM = 0


def kname(key):
    return key[0] if isinstance(key, tuple) else key


class Op:
    __slots__ = ("eng", "fn", "reads", "writes", "dma", "deps", "sig", "need", "idx")

    def __init__(self, eng, fn, reads, writes, dma):
        self.eng = eng
        self.fn = fn
        self.reads = tuple(reads)
        self.writes = tuple(writes)
        self.dma = dma
        self.deps = ()
        self.sig = None
        self.need = False


class Prog:
    ENGS = ("pe", "act", "dve", "pool", "sp")
    N_DMA_SEM = {"sp": 40, "pool": 24, "act": 8}

    def __init__(self):
        self.ops = []
        self.alias = {}
        self.unwritten = set()

    def add(self, eng, fn, reads=(), writes=(), dma=False):
        op = Op(eng, fn, reads, writes, dma)
        op.idx = len(self.ops)
        self.ops.append(op)
        return op

    def emit(self, nc, stack, final_ops):
        ops = self.ops
        last_w = {}
        readers = {}
        touch = {}
        touch_dma = {}
        synced = set()
        for i, op in enumerate(ops):
            deps = set()
            for r in op.reads:
                if r in last_w:
                    deps.add(last_w[r])
                else:
                    self.unwritten.add(r)
            for w in op.writes:
                if w in last_w:
                    deps.add(last_w[w])
                for rd in readers.get(w, ()):
                    deps.add(rd)
            for key in op.reads + op.writes:
                nm = kname(key)
                if nm in self.alias and (op.eng, nm) not in synced:
                    synced.add((op.eng, nm))
                    for old in self.alias[nm]:
                        for _, oi in touch.get(old, {}).items():
                            deps.add(oi)
                        for oi in touch_dma.get(old, ()):
                            deps.add(oi)
            deps.discard(i)
            op.deps = deps
            for r in op.reads:
                readers.setdefault(r, []).append(i)
            for w in op.writes:
                last_w[w] = i
                readers[w] = []
            for key in op.reads + op.writes:
                nm = kname(key)
                if op.dma:
                    touch_dma.setdefault(nm, []).append(i)
                else:
                    touch.setdefault(nm, {})[op.eng] = i
        dma_slots = {q: [None] * n for q, n in self.N_DMA_SEM.items()}
        dma_ctr = {q: 0 for q in self.N_DMA_SEM}
        dma_slot_of = {}
        for i, op in enumerate(ops):
            if op.dma:
                q = op.eng
                s = dma_ctr[q] % self.N_DMA_SEM[q]
                dma_ctr[q] += 1
                prev = dma_slots[q][s]
                if prev is not None:
                    op.deps.add(prev)
                dma_slots[q][s] = i
                dma_slot_of[i] = s
        fin = self.add("sp", None, (), ())
        fin.deps = set(o.idx for o in final_ops)
        for op in ops:
            nd = set()
            best = {}
            for d in op.deps:
                dop = ops[d]
                if dop.dma:
                    nd.add(d)
                    continue
                if dop.eng == op.eng and op.eng == "pe" and not op.dma:
                    continue
                if dop.eng not in best or best[dop.eng] < d:
                    best[dop.eng] = d
            nd.update(best.values())
            for d in nd:
                ops[d].need = True
            op.deps = nd
        eng_sem = {e: stack.enter_context(nc.semaphore("sem_" + e)) for e in self.ENGS}
        dma_sem = {q: [stack.enter_context(nc.semaphore(f"dsem_{q}{j}")) for j in range(n)]
                   for q, n in self.N_DMA_SEM.items()}
        eng_cnt = {e: 0 for e in self.ENGS}
        dma_use = {q: [0] * n for q, n in self.N_DMA_SEM.items()}
        for i, op in enumerate(ops):
            if op.dma:
                s = dma_slot_of[i]
                dma_use[op.eng][s] += 1
                op.sig = (dma_sem[op.eng][s], 16 * dma_use[op.eng][s], 16)
            elif op.need:
                eng_cnt[op.eng] += 1
                op.sig = (eng_sem[op.eng], eng_cnt[op.eng], 1)
        per_eng = {e: [op for op in ops if op.eng == e] for e in self.ENGS}
        self.stats = {e: len(per_eng[e]) for e in self.ENGS}
        self.stats["sig"] = dict(eng_cnt)
        self.stats["unwritten"] = sorted(str(u) for u in self.unwritten)

        def run(ename, eng):
            waited = {}
            nwait = 0
            for op in per_eng[ename]:
                need = {}
                for d in op.deps:
                    sem, val, _ = ops[d].sig
                    k = id(sem)
                    if waited.get(k, 0) >= val:
                        continue
                    if k not in need or need[k][1] < val:
                        need[k] = (sem, val)
                for k, (sem, val) in need.items():
                    eng.wait_ge(sem, val)
                    waited[k] = val
                    nwait += 1
                if op.fn is None:
                    continue
                inst = op.fn(eng)
                if op.sig is not None:
                    inst.then_inc(op.sig[0], op.sig[2])
            self.stats["wait_" + ename] = nwait

        with nc.Block() as block:
            @block.tensor
            def _(e):
                run("pe", e)

            @block.scalar
            def _(e):
                run("act", e)

            @block.vector
            def _(e):
                run("dve", e)

            @block.gpsimd
            def _(e):
                run("pool", e)

            @block.sync
            def _(e):
                run("sp", e)


class Arena:
    def __init__(self, prog, tensor, nbytes):
        self.P = prog
        self.t = tensor
        self.free = [(0, nbytes)]
        self.live = {}
        self.dead = []
        self.peak = 0

    def alloc(self, name, shape, dt):
        esz = 4 if dt == F32 else 2
        n = 1
        for s in shape[1:]:
            n *= s
        nbytes = (n * esz + 63) // 64 * 64
        for idx, (o, sz) in enumerate(self.free):
            if sz >= nbytes:
                off = o
                if sz == nbytes:
                    self.free.pop(idx)
                else:
                    self.free[idx] = (o + nbytes, sz - nbytes)
                break
        else:
            raise RuntimeError(f"arena full allocating {name} {nbytes}B; live={ {k: v[1] for k, v in self.live.items()} }")
        assert name not in self.live and name not in self.P.alias
        self.live[name] = (off, nbytes)
        self.peak = max(self.peak, off + nbytes)
        al = [d[0] for d in self.dead if d[1] < off + nbytes and off < d[1] + d[2]]
        self.P.alias[name] = al
        v = self.t[0:shape[0], off // 2:(off + n * esz) // 2]
        if dt == F32:
            v = v.bitcast(F32)
        if len(shape) == 3:
            v = v.rearrange("p (a b) -> p a b", b=shape[2])
        elif len(shape) == 4:
            v = v.rearrange("p (a b c) -> p a b c", b=shape[2], c=shape[3])
        return v

    def release(self, *names):
        for name in names:
            off, nbytes = self.live.pop(name)
            self.dead.append((name, off, nbytes))
            self.free.append((off, nbytes))
        self.free.sort()
        merged = []
        for o, sz in self.free:
            if merged and merged[-1][0] + merged[-1][1] == o:
                merged[-1] = (merged[-1][0], merged[-1][1] + sz)
            else:
                merged.append((o, sz))
        self.free = merged


def build(stage=99, dbg=False):
    nc = bass.Bass("TRN2", target_bir_lowering=False)
    P = Prog()
    st = ExitStack()

    def din(name, shape, dt=F32):
        return nc.dram_tensor(name, list(shape), dt, kind="ExternalInput").ap()

    x_all = din("x_all", [S, D])
    x_own = din("x_own", [TOK_OWN, D])
    x_halo = din("x_halo", [64, D])
    halo_mask = din("halo_mask", [128, 64])
    invcnt = din("invcnt", [128, 64])
    maskb = din("maskb", [128, 8 * 512])
    c_in = din("c_in", [128, 8])
    w_ada = din("w_ada", [D, 6 * D])
    b_ada = din("b_ada", [1, 6 * D])
    b_adaT = din("b_adaT", [128, 48])
    g_norm1 = din("g_norm1T", [128, 8])
    w_in = din("w_in", [D, 4096])
    g_q = din("g_q2", [128, 1])
    g_k = din("g_k2", [128, 1])
    w_sb_out = din("w_sb_out", [512, D])
    w_pool_group = din("w_pool_group", [4, 128, 128])
    pool_scale = din("pool_scaleT", [128, 4])
    w_pool_out = din("w_pool_out", [512, D])
    w_out = din("w_out", [D, D])
    g_norm2 = din("g_norm2T", [128, 8])
    w_router = din("w_router", [D, 64])
    router_bias = din("router_bias", [1, 64])
    w_exp_gate = din("w_exp_gate", [64, D, 128])
    w_exp_up = din("w_exp_up", [64, D, 128])
    w_exp_down = din("w_exp_down", [64, 128, D])
    w_sh_gate = din("w_sh_gate", [D, 256])
    w_sh_up = din("w_sh_up", [D, 256])
    w_sh_down = din("w_sh_down", [256, D])
    c_ident = din("c_ident", [128, 128])
    c_ntri = din("c_ntri", [128, 128])
    c_blk = din("c_blk", [128, 128])
    c_sel = din("c_sel", [64, 64 * 128])

    out = nc.dram_tensor("out", [TOK_OWN, D], F32, kind="ExternalOutput").ap()
    mod_scr = nc.dram_tensor("mod_scr", [1, 2 * D], F32, kind="Internal").ap()
    hT_scr = nc.dram_tensor("hT_scr", [NT_OWN, 128, 8 * 512], BF16, kind="Internal").ap()
    xnew_scr = nc.dram_tensor("xnew_scr", [TOK_OWN, D], F32, kind="Internal").ap()
    gates_scr = nc.dram_tensor("gates_scr", [64, TOK_OWN], BF16, kind="Internal").ap()
    dbg_out = None
    if dbg:
        dbg_out = nc.dram_tensor("dbg", [128, 16384], F32, kind="ExternalOutput").ap()

    arena_t = st.enter_context(nc.sbuf_tensor("arena", [128, ARENA_BYTES // 2], BF16))
    AR = Arena(P, arena_t, ARENA_BYTES)
    sb = AR.alloc

    psum = st.enter_context(nc.psum_tensor("psum", [128, 8 * 512], F32))

    def bank(i):
        return psum[:, i * 512:(i + 1) * 512]

    def bank_bf(i):
        return psum[:, i * 512:(i + 1) * 512].bitcast(BF16)

    final_ops = []

    def dma(q, out_ap, in_ap, reads=(), writes=()):
        return P.add(q, lambda e: e.dma_start(out=out_ap, in_=in_ap), reads=reads, writes=writes, dma=True)

    ident = sb("ident", [128, 128], BF16)
    ntri = sb("ntri", [128, 128], BF16)
    nones = sb("nones", [128, 128], BF16)
    blk = sb("blk", [128, 128], BF16)
    eps_col = sb("eps", [128, 1], F32)
    one_col = sb("one", [128, 1], F32)
    dma("pool", ident, c_ident, writes=["ident"])
    dma("pool", ntri, c_ntri, writes=["ntri"])
    dma("pool", blk, c_blk, writes=["blk"])
    P.add("dve", lambda e: e.memset(nones, -1.0), writes=["nones"])
    P.add("dve", lambda e: e.memset(eps_col, EPS), writes=["eps"])
    P.add("dve", lambda e: e.memset(one_col, 1.0), writes=["one"])
    deps_col = sb("deps", [128, 1], F32)
    mhalf_col = sb("mhalf", [128, 1], F32)
    P.add("dve", lambda e: e.memset(deps_col, D * EPS), writes=["deps"])
    P.add("dve", lambda e: e.memset(mhalf_col, -0.5), writes=["mhalf"])

    c_sb = sb("c_sb", [128, 8], F32)
    c_bf = sb("c_bf", [128, 8], BF16)
    modT = sb("modT", [128, 48], F32)
    bT = sb("bT", [128, 48], F32)
    g1T = sb("g1T", [128, 8], F32)
    g2T = sb("g2T", [128, 8], F32)
    A1 = sb("A1", [128, 8], F32)
    A2 = sb("A2", [128, 8], F32)
    gate1_bc = sb("gate1_bc", [128, D], F32)
    gate2_bc = sb("gate2_bc", [128, D], F32)
    dma("sp", c_sb, c_in, writes=["c_sb"])
    dma("sp", bT, b_adaT, writes=["bT"])
    dma("sp", g1T, g_norm1, writes=["g1T"])
    dma("sp", g2T, g_norm2, writes=["g2T"])
    P.add("act", lambda e: e.activation(out=c_bf, in_=c_sb, func=AF.Silu), reads=["c_sb"], writes=["c_bf"])
    wada_v = w_ada.rearrange("(k p) n -> p k n", p=128)
    wada_t = [sb(f"wada{i}", [128, 8, 512], BF16) for i in range(2)]
    brow_t = [sb(f"brow{i}", [1, 512], F32) for i in range(2)]
    mrow_t = [sb(f"mrow{i}", [1, 512], F32) for i in range(2)]
    ada_cnt = {"n": 0}

    def adaln_tile(j12):
        i = ada_cnt["n"]; ada_cnt["n"] += 1
        t = wada_t[i % 2]; key = ("wada%d" % (i % 2),)
        dma("pool", t, wada_v[:, :, j12 * 512:(j12 + 1) * 512], writes=[key])
        is_gate = j12 in (4, 5, 10, 11)
        if not is_gate:
            for jj in range(4):
                j = j12 * 4 + jj
                for k in range(8):
                    P.add("pe", lambda e, k=k, j=j, jj=jj: e.matmul(bank(7)[:, j:j + 1], lhsT=t[:, k, jj * 128:(jj + 1) * 128],
                                                                    rhs=c_bf[:, k:k + 1], start=(k == 0), stop=(k == 7)),
                          reads=[key, "c_bf"], writes=[("ps", 7)])
            j0 = j12 * 4
            P.add("dve", lambda e: e.tensor_tensor(out=modT[:, j0:j0 + 4], in0=bank(7)[:, j0:j0 + 4], in1=bT[:, j0:j0 + 4], op=ALU.add),
                  reads=[("ps", 7), "bT"], writes=[("modT", j0 + jj) for jj in range(4)])
        else:
            br = brow_t[i % 2]; mr = mrow_t[i % 2]
            gi = {4: 0, 5: 1, 10: 2, 11: 3}[j12]
            dma("sp", br, b_ada[0:1, j12 * 512:(j12 + 1) * 512], writes=[("brow%d" % (i % 2),)])
            for k in range(8):
                P.add("pe", lambda e, k=k: e.matmul(bank(6)[0:1, :], lhsT=c_bf[:, k:k + 1], rhs=t[:, k, :],
                                                    start=(k == 0), stop=(k == 7)),
                      reads=[key, "c_bf"], writes=[("ps", 6)])
            P.add("dve", lambda e: e.tensor_tensor(out=mr, in0=bank(6)[0:1, :], in1=br, op=ALU.add),
                  reads=[("ps", 6), ("brow%d" % (i % 2),)], writes=[("mrow%d" % (i % 2),)])
            dma("sp", mod_scr[0:1, gi * 512:(gi + 1) * 512], mr, reads=[("mrow%d" % (i % 2),)], writes=[("mod_scr", gi)])

    for j12 in (0, 1, 2, 3):
        adaln_tile(j12)
    P.add("dve", lambda e: e.scalar_tensor_tensor(out=A1, in0=modT[:, 8:16], scalar=1.0, in1=g1T,
                                                  op0=ALU.add, op1=ALU.mult),
          reads=[("modT", j) for j in range(8, 16)] + ["g1T"], writes=["A1"])

    def B1(c):
        return modT[:, c:c + 1]

    def B2(c):
        return modT[:, 24 + c:25 + c]
    B1keys = [("modT", j) for j in range(8)]
    B2keys = [("modT", j) for j in range(24, 32)]

    xs_t = [sb(f"xs{i}", [128, D], F32) for i in range(3)]
    xn_t = [sb(f"xn{i}", [128, D], BF16) for i in range(3)]
    NXN = {"n": 3}
    junk = sb("junk", [128, D], BF16)
    stat = sb("stat", [128, 64], F32)
    cnt = {"xs": 0, "xn": 0, "st": 0, "tp": 0, "hT": 0}

    def rms_scale_block(xs, kx, nrows):
        i2 = cnt["xn"]; cnt["xn"] += 1
        xn = xn_t[i2 % 3]; kn = ("xn%d" % (i2 % 3),)
        s = cnt["st"] % 16; cnt["st"] += 1
        ks = ("stat", s)
        P.add("act", lambda e: e.activation(out=junk[0:nrows, :], in_=xs[0:nrows, :], func=AF.Square,
                                            accum_out=stat[0:nrows, 4 * s:4 * s + 1]),
              reads=[kx], writes=["junk", ks])
        P.add("act", lambda e: e.activation(out=stat[0:nrows, 4 * s + 1:4 * s + 2], in_=stat[0:nrows, 4 * s:4 * s + 1],
                                            func=AF.Ln, scale=1.0 / D, bias=eps_col[0:nrows, :]),
              reads=[ks, "eps"], writes=[ks])
        P.add("act", lambda e: e.activation(out=stat[0:nrows, 4 * s + 2:4 * s + 3], in_=stat[0:nrows, 4 * s + 1:4 * s + 2],
                                            func=AF.Exp, scale=-0.5),
              reads=[ks], writes=[ks])
        P.add("dve", lambda e: e.tensor_scalar(out=xn[0:nrows, :], in0=xs[0:nrows, :],
                                               scalar1=stat[0:nrows, 4 * s + 2:4 * s + 3], scalar2=None, op0=ALU.mult),
              reads=[kx, ks], writes=[kn])
        return xn, kn

    def transpose_A(xn, kn, nrows, tp_banks):
        tb = tp_banks[cnt["tp"] % len(tp_banks)]; cnt["tp"] += 1
        ktp = ("ps", tb)
        tpb = bank_bf(tb)
        for c in range(8):
            P.add("pe", lambda e, c=c: e.transpose(out=tpb[:, c * 128:c * 128 + nrows],
                                                   in_=xn[0:nrows, c * 128:(c + 1) * 128],
                                                   identity=ident[0:nrows, 0:nrows]),
                  reads=[kn, "ident"], writes=[ktp])
        return tpb, ktp

    def transpose_B(tpb, ktp, nrows, hT, hkey, col0, Acols, Akey, Bfn, Bkeys, act_share=4):
        for c in range(8):
            dst = hT[:, c, col0:col0 + nrows]
            src = tpb[:, c * 128:c * 128 + nrows]
            if c < act_share:
                P.add("act", lambda e, c=c, dst=dst, src=src: e.activation(out=dst, in_=src, func=AF.Identity,
                                                                            scale=Acols[:, c:c + 1], bias=Bfn(c)),
                      reads=[ktp, Akey] + Bkeys, writes=[hkey])
            else:
                P.add("dve", lambda e, c=c, dst=dst, src=src: e.tensor_scalar(out=dst, in0=src, scalar1=Acols[:, c:c + 1],
                                                                               scalar2=Bfn(c), op0=ALU.mult, op1=ALU.add),
                      reads=[ktp, Akey] + Bkeys, writes=[hkey])

    def transpose_mod(xn, kn, nrows, hT, hkey, col0, Acols, Akey, Bfn, Bkeys, tp_banks):
        tpb, ktp = transpose_A(xn, kn, nrows, tp_banks)
        transpose_B(tpb, ktp, nrows, hT, hkey, col0, Acols, Akey, Bfn, Bkeys, act_share=6)

    def norm_A1(src_ap, nrows):
        i = cnt["xs"]; cnt["xs"] += 1
        xs = xs_t[i % 3]; kx = ("xs%d" % (i % 3),)
        dma("sp", xs[0:nrows, :], src_ap, writes=[kx])
        return rms_scale_block(xs, kx, nrows)

    def norm_A2(ctx, nrows, tp_banks=(0, 1)):
        return transpose_A(ctx[0], ctx[1], nrows, tp_banks)

    def norm_B(ctx, nrows, hT, hkey, col0):
        transpose_B(ctx[0], ctx[1], nrows, hT, hkey, col0, A1, "A1", B1, B1keys, act_share=1)

    def run_blocks(items, skew1=2, skew2=0):
        blks = [it for it in items if it[0] == "blk"]
        c1 = {}
        c2 = {}
        st_ = {"n1": 0, "n2": 0}

        def ensure1(upto):
            while st_["n1"] <= upto and st_["n1"] < len(blks):
                b = blks[st_["n1"]]
                c1[st_["n1"]] = norm_A1(b[1], b[2])
                st_["n1"] += 1

        def ensure2(upto):
            while st_["n2"] <= upto and st_["n2"] < len(blks):
                n = st_["n2"]
                ensure1(n)
                c2[n] = norm_A2(c1.pop(n), blks[n][2])
                st_["n2"] += 1
        bi = 0
        for it in items:
            if it[0] == "blk":
                ensure1(bi + skew1)
                ensure2(bi + skew2)
                norm_B(c2.pop(bi), it[2], it[3], it[4], it[5])
                bi += 1
            else:
                it[1]()

    win_v = w_in.rearrange("(k p) n -> p k n", p=128)
    wqkvu = sb("wqkvu", [128, 8, 2048], BF16)
    for j in (1, 2, 0, 3):
        dma("pool", wqkvu[:, :, j * 512:(j + 1) * 512], win_v[:, :, j * 512:(j + 1) * 512], writes=[("wqkvu", j)])
    gq_col = sb("gq_raw", [128, 1], F32)
    gk_col = sb("gk", [128, 1], F32)
    gqs_col = sb("gq", [128, 1], F32)
    dma("sp", gq_col, g_q, writes=["gq_raw"])
    dma("sp", gk_col, g_k, writes=["gk"])
    P.add("dve", lambda e: e.tensor_scalar(out=gqs_col, in0=gq_col, scalar1=0.125, scalar2=None, op0=ALU.mult),
          reads=["gq_raw"], writes=["gq"])
    gk8_col = sb("gk8", [128, 1], F32)
    P.add("dve", lambda e: e.tensor_scalar(out=gk8_col, in0=gk_col, scalar1=8.0, scalar2=None, op0=ALU.mult),
          reads=["gk"], writes=["gk8"])

    KT = sb("KT", [128, 4, S], BF16)
    V = sb("V", [128, 32, 512], BF16)
    hT_t = [sb(f"hT{i}", [128, 8, 512], BF16) for i in range(2)]
    sq_t = [sb(f"sq{i}", [128, 512], BF16) for i in range(2)]
    ln_t = [sb(f"lt{i}", [128, 512], F32) for i in range(2)]
    rs_t = [sb(f"rs{i}", [128, 512], F32) for i in range(2)]
    cq = {"n": 0}

    def headnorm_p1(hT, hkey, wcol0, wkey, hp):
        i = cq["n"]; cq["n"] += 1
        pb = (2, 3, 5)[i % 3]
        kpb = ("ps", pb)
        sq = sq_t[i % 2]
        ksq = ("sq%d" % (i % 2),)
        for k in range(8):
            P.add("pe", lambda e, k=k: e.matmul(bank(pb), lhsT=wqkvu[:, k, wcol0 + hp * 128:wcol0 + (hp + 1) * 128],
                                                rhs=hT[:, k, :], start=(k == 0), stop=(k == 7)),
                  reads=[wkey, hkey], writes=[kpb])
        P.add("act", lambda e: e.activation(out=sq, in_=bank(pb), func=AF.Square), reads=[kpb], writes=[ksq])
        return (i, hp)

    def headnorm_p2(ctx, gcol, gkey, dst, dkey_fn, col0):
        i, hp = ctx
        pb = (2, 3, 5)[i % 3]
        kpb = ("ps", pb)
        kss = ("ps", 4)
        ssb = bank(4)
        sq = sq_t[i % 2]; lt = ln_t[i % 2]; rs = rs_t[i % 2]
        ksq = ("sq%d" % (i % 2),); klt = ("lt%d" % (i % 2),); krs = ("rs%d" % (i % 2),)
        P.add("pe", lambda e: e.matmul(ssb, lhsT=blk, rhs=sq, start=True, stop=True), reads=[ksq, "blk"], writes=[kss])
        P.add("act", lambda e: e.activation(out=lt, in_=ssb, func=AF.Ln, scale=1.0 / 64, bias=eps_col),
              reads=[kss, "eps"], writes=[klt])
        P.add("act", lambda e: e.activation(out=rs, in_=lt, func=AF.Exp, scale=-0.5), reads=[klt], writes=[krs])
        P.add("dve", lambda e: e.scalar_tensor_tensor(out=dst[:, hp, col0:col0 + 512], in0=bank(pb),
                                                      scalar=gcol[:, 0:1], in1=rs, op0=ALU.mult, op1=ALU.mult),
              reads=[kpb, krs, gkey], writes=[dkey_fn(hp)])

    items = []
    n_all_tiles = 8 if stage >= 1 else 0

    hn_ctx = {}

    def piece1a(T, i, hT, hkey):
        hn_ctx[("k", T, i)] = headnorm_p1(hT, hkey, 512, ("wqkvu", 1), i)
        vb = 6
        for k in range(8):
            P.add("pe", lambda e, k=k: e.matmul(bank(vb), lhsT=hT[:, k, i * 128:(i + 1) * 128],
                                                rhs=wqkvu[:, k, 1024:1536], start=(k == 0), stop=(k == 7)),
                  reads=[("wqkvu", 2), hkey], writes=[("ps", vb)])
        P.add("dve", lambda e: e.tensor_copy(out=V[:, T * 4 + i, :], in_=bank(vb)),
              reads=[("ps", vb)], writes=[("V", T * 4 + i)])
        if i == 3:
            adaln_tile(4 + T)

    def piece1b(T, i):
        headnorm_p2(hn_ctx.pop(("k", T, i)), gk_col, "gk", KT, lambda hp: ("KT", hp, T), T * 512)

    pieces1 = []
    blk_items1 = []
    for T in range(n_all_tiles):
        hT = hT_t[cnt["hT"] % 2]; hkey = ("hT%d" % (cnt["hT"] % 2),); cnt["hT"] += 1
        for b4 in range(4):
            r0 = T * 512 + b4 * 128
            blk_items1.append(("blk", x_all[r0:r0 + 128, :], 128, hT, hkey, b4 * 128))
            pieces1.append((T, b4, hT, hkey))
    for n in range(len(blk_items1) + 5):
        if n < len(blk_items1):
            items.append(blk_items1[n])
        if 0 <= n - 4 < len(pieces1):
            items.append(("fn", lambda a=pieces1[n - 4]: piece1a(*a)))
        if 0 <= n - 5 < len(pieces1):
            items.append(("fn", lambda a=pieces1[n - 5]: piece1b(a[0], a[1])))

    def post_ada():
        P.add("dve", lambda e: e.scalar_tensor_tensor(out=A2, in0=modT[:, 32:40], scalar=1.0, in1=g2T,
                                                      op0=ALU.add, op1=ALU.mult),
              reads=[("modT", j) for j in range(32, 40)] + ["g2T"], writes=["A2"])
        dma("sp", gate1_bc, mod_scr[0:1, 0:D].to_broadcast([128, D]), reads=[("mod_scr", 0), ("mod_scr", 1)], writes=["gate1_bc"])
        dma("sp", gate2_bc, mod_scr[0:1, D:2 * D].to_broadcast([128, D]), reads=[("mod_scr", 2), ("mod_scr", 3)], writes=["gate2_bc"])

    if stage >= 1:
        items.append(("fn", post_ada))
    if stage < 2:
        run_blocks(items)

    if stage >= 2:
        AR.release("wada0", "wada1", "brow0", "brow1", "mrow0", "mrow1")
        QT = sb("QT", [128, 4, TOK_OWN], BF16)
        pooledT = sb("pooledT", [128, 4, TOK_OWN], BF16)
        wpg = sb("wpg", [128, 4, 128], BF16)
        pscale = sb("pscale", [128, 4], F32)
        hmask = sb("hmask", [128, 64], F32)
        icnt = sb("icnt", [128, 64], F32)
        uhalo = sb("uhalo", [128, 4, 64], F32)
        hTh = sb("hTh", [128, 8, 64], BF16)
        uext = [sb(f"uext{i}", [128, 528], F32) for i in range(2)]
        sAB = [sb("sA", [128, 528], F32), sb("sB", [128, 528], F32)]
        mixed = [sb(f"mixed{i}", [128, 512], BF16) for i in range(2)]
        tmp16 = sb("tmp16", [128, 16], F32)
        def setup2():
            dma("pool", wpg, w_pool_group.rearrange("g c d -> c g d"), writes=["wpg"])
            dma("sp", pscale, pool_scale, writes=["pscale"])
            dma("sp", hmask, halo_mask, writes=["hmask"])
            dma("sp", icnt, invcnt, writes=["icnt"])
            P.add("pool", lambda e: e.memset(sAB[0], 0.0), writes=["sA"])
            P.add("pool", lambda e: e.memset(sAB[1], 0.0), writes=["sB"])

        def halo_proj():
            for g in range(4):
                ub = 2 + g % 2
                for k in range(8):
                    P.add("pe", lambda e, k=k, g=g, ub=ub: e.matmul(bank(ub)[:, 0:64], lhsT=wqkvu[:, k, 1536 + g * 128:1536 + (g + 1) * 128],
                                                                    rhs=hTh[:, k, 0:64], start=(k == 0), stop=(k == 7)),
                          reads=[("wqkvu", 3), ("hTh",)], writes=[("ps", ub)])
                P.add("dve", lambda e, g=g, ub=ub: e.tensor_tensor(out=uhalo[:, g, :], in0=bank(ub)[:, 0:64], in1=hmask, op=ALU.mult),
                      reads=[("ps", ub), "hmask"], writes=[("uhalo", g)])

        pend_wpg = []

        def flush_wpg():
            while pend_wpg:
                g, kt, mx, kmx = pend_wpg.pop(0)
                gb_ = 7
                P.add("pe", lambda e, g=g, mx=mx, gb_=gb_: e.matmul(bank(gb_), lhsT=wpg[:, g, :], rhs=mx, start=True, stop=True),
                      reads=["wpg", kmx], writes=[("ps", gb_)])
                P.add("act", lambda e, g=g, gb_=gb_, kt=kt: e.activation(out=pooledT[:, g, kt * 512:(kt + 1) * 512], in_=bank(gb_),
                                                                         func=AF.Copy, scale=pscale[:, g:g + 1]),
                      reads=[("ps", gb_), "pscale"], writes=[("pooledT", g, kt)])

        def piece2b(kt, g):
            headnorm_p2(hn_ctx.pop(("q", kt, g)), gqs_col, "gq", QT, lambda hp: ("QT", hp, kt), kt * 512)

        def piece2a(kt, g, hT, hkey):
            if g == 0:
                dma("sp", hT_scr[kt], hT.rearrange("p a b -> p (a b)"), reads=[hkey], writes=[("hT_scr", kt)])
            hn_ctx[("q", kt, g)] = headnorm_p1(hT, hkey, 0, ("wqkvu", 0), g)
            ue = uext[g % 2]; kue = ("uext%d" % (g % 2),)
            ub = 6
            for k in range(8):
                P.add("pe", lambda e, k=k: e.matmul(bank(ub), lhsT=wqkvu[:, k, 1536 + g * 128:1536 + (g + 1) * 128],
                                                    rhs=hT[:, k, :], start=(k == 0), stop=(k == 7)),
                      reads=[("wqkvu", 3), hkey], writes=[("ps", ub)])
            flush_wpg()
            P.add("act", lambda e: e.activation(out=ue[:, 16:528], in_=bank(ub), func=AF.Copy),
                  reads=[("ps", ub)], writes=[kue])
            P.add("pool", lambda e: e.tensor_copy(out=ue[:, 0:16], in_=uhalo[:, g, kt * 16:(kt + 1) * 16]),
                  reads=[("uhalo", g)], writes=[kue])
            cur = ue; kcur = kue
            for j in range(g + 1):
                dst = sAB[j % 2]; kd = "sA" if j % 2 == 0 else "sB"
                sh = 1 << j
                P.add("pool", lambda e, dst=dst, cur=cur, sh=sh: e.tensor_tensor(out=dst[:, sh:528], in0=cur[:, sh:528],
                                                                                in1=cur[:, 0:528 - sh], op=ALU.add),
                      reads=[kcur], writes=[kd])
                cur = dst; kcur = kd
            W = float(1 << (g + 1))
            mx = mixed[g % 2]; kmx = ("mixed%d" % (g % 2),)
            P.add("dve", lambda e, cur=cur: e.scalar_tensor_tensor(out=mx, in0=cur[:, 16:528], scalar=1.0 / W,
                                                                   in1=ue[:, 16:528], op0=ALU.mult, op1=ALU.subtract),
                  reads=[kcur, kue], writes=[kmx])
            if kt == 0:
                P.add("dve", lambda e, cur=cur: e.tensor_tensor(out=tmp16, in0=cur[:, 16:32], in1=icnt[:, g * 16:(g + 1) * 16], op=ALU.mult),
                      reads=[kcur, "icnt"], writes=["tmp16"])
                P.add("dve", lambda e: e.tensor_tensor(out=mx[:, 0:16], in0=tmp16, in1=ue[:, 16:32], op=ALU.subtract),
                      reads=["tmp16", kue], writes=[kmx])
            pend_wpg.append((g, kt, mx, kmx))

        items.append(("fn", setup2))
        items.append(("blk", x_halo[0:64, :], 64, hTh, ("hTh",), 0))
        items.append(("fn", halo_proj))
        pieces2 = []
        blk_items2 = []
        for kt in range(NT_OWN):
            hT = hT_t[cnt["hT"] % 2]; hkey = ("hT%d" % (cnt["hT"] % 2),); cnt["hT"] += 1
            for b4 in range(4):
                r0 = kt * 512 + b4 * 128
                blk_items2.append(("blk", x_own[r0:r0 + 128, :], 128, hT, hkey, b4 * 128))
                pieces2.append((kt, b4, hT, hkey))
        for n in range(len(blk_items2) + 5):
            if n < len(blk_items2):
                items.append(blk_items2[n])
            if 0 <= n - 4 < len(pieces2):
                items.append(("fn", lambda a=pieces2[n - 4]: piece2a(*a)))
            if 0 <= n - 5 < len(pieces2):
                items.append(("fn", lambda a=pieces2[n - 5]: piece2b(a[0], a[1])))
        items.append(("fn", flush_wpg))
        run_blocks(items)

    if stage >= 3:
        AR.release("wqkvu", "xs0", "xs1", "xs2", "xn0", "xn1", "xn2", "junk", "hT0", "hT1", "sq0", "sq1", "lt0", "lt1", "rs0", "rs1",
                   "uext0", "uext1", "sA", "sB", "mixed0", "mixed1", "tmp16", "hTh", "uhalo", "wpg", "hmask", "icnt")
        wsb = sb("wsb", [128, 4, D], BF16)
        wpo = sb("wpo", [128, 4, D], BF16)
        wout = sb("wout", [128, 8, D], BF16)
        wr = sb("wr", [128, 8, 64], BF16)
        rbias = sb("rbias", [128, 64], F32)
        mkb = sb("mkb", [128, 8, 512], BF16)
        dma("pool", mkb, maskb.rearrange("p (a b) -> p a b", b=512), writes=["mkb"])
        dma("pool", wout, w_out.rearrange("(c p) d -> p c d", p=128), writes=["wout"])
        dma("pool", wsb, w_sb_out.rearrange("(h p) d -> p h d", p=128), writes=["wsb"])
        dma("pool", wpo, w_pool_out.rearrange("(h p) d -> p h d", p=128), writes=["wpo"])
        dma("pool", wr, w_router.rearrange("(c p) e -> p c e", p=128), writes=["wr"])
        dma("sp", rbias, router_bias[0:1, :].to_broadcast([128, 64]), writes=["rbias"])
        for c in range(8):
            P.add("pool", lambda e, c=c: e.tensor_tensor(out=wout[:, c, :], in0=wout[:, c, :], in1=gate1_bc, op=ALU.mult),
                  reads=["wout", "gate1_bc"], writes=["wout"])
        oT = sb("oT", [128, 4, TOK_OWN], BF16)
        eG = [sb(f"eG{g}", [128, 1024], F32) for g in range(2)]
        spG = [sb(f"spG{g}", [128, 1024], BF16) for g in range(2)]
        aG = [sb(f"aG{g}", [128, 1024], BF16) for g in range(2)]
        SbG = [[sb(f"SbG{g}{s_}", [128, 1024], BF16) for s_ in range(2)] for g in range(2)]

        def PGb(j, g):
            if g == 0:
                return (0, 1) if j % 2 == 0 else (6, 7)
            return (2, 3)

        def PGv(j, g):
            b0 = PGb(j, g)[0]
            return psum[:, b0 * 512:(b0 + 2) * 512]
        grp_i = 0
        n_kt = NT_OWN if stage >= 4 or not dbg else 2
        for kt in range(n_kt):
            nst = 8 * (kt + 1)
            for hq in range(2):
                obase = 4
                grp_i += 1

                def DUM(n):
                    for q in range(n):
                        P.add("pe", lambda e, q=q: e.matmul(bank(6 + q % 2), lhsT=ident, rhs=mkb[:, q % 8, :], start=True, stop=True),
                              reads=["ident", "mkb"], writes=[("ps", 6 + q % 2)])

                def Zg(j, g, kt=kt, hq=hq):
                    kb = 8 * kt + 7 - j
                    hp = 2 * hq + g
                    masked = j < 8
                    for c in range(2):
                        pb = PGb(j, g)[c]
                        P.add("pe", lambda e, c=c, pb=pb: e.matmul(bank(pb), lhsT=KT[64 * c:64 * c + 64, hp, kb * 128:(kb + 1) * 128],
                                                                   rhs=QT[64 * c:64 * c + 64, hp, kt * 512:(kt + 1) * 512],
                                                                   start=True, stop=(not masked)),
                              reads=[("KT", hp, kb // 4), ("QT", hp, kt)], writes=[("ps", pb)])
                        if masked:
                            P.add("pe", lambda e, pb=pb: e.matmul(bank(pb), lhsT=ident, rhs=mkb[:, j, :], start=False, stop=True),
                                  reads=["ident", "mkb"], writes=[("ps", pb)])

                def Eg(j, g):
                    P.add("act", lambda e: e.activation(out=eG[g], in_=PGv(j, g), func=AF.Exp),
                          reads=[("ps", PGb(j, g)[0]), ("ps", PGb(j, g)[1])], writes=[(f"eG{g}",)])

                def Lg(j, g):
                    P.add("act", lambda e: e.activation(out=spG[g], in_=eG[g], func=AF.Ln, bias=one_col),
                          reads=[(f"eG{g}",), "one"], writes=[(f"spG{g}",)])

                def NOg(j, g):
                    if j == 0:
                        return
                    sbf = SbG[g][(j - 1) % 2]
                    for c in range(2):
                        pb = PGb(j, g)[c]
                        P.add("pe", lambda e, c=c, pb=pb, sbf=sbf: e.matmul(bank(pb), lhsT=nones, rhs=sbf[:, c * 512:(c + 1) * 512],
                                                                            start=False, stop=False, skip_group_check=True),
                              reads=["nones", (f"SbG{g}{(j - 1) % 2}",)], writes=[("ps", pb)])

                def TRg(j, g):
                    for c in range(2):
                        pb = PGb(j, g)[c]
                        P.add("pe", lambda e, c=c, pb=pb: e.matmul(bank(pb), lhsT=ntri, rhs=spG[g][:, c * 512:(c + 1) * 512],
                                                                   start=False, stop=True, skip_group_check=True),
                              reads=["ntri", (f"spG{g}",)], writes=[("ps", pb)])

                def SUg(j, g, nst=nst):
                    if j >= nst - 1:
                        return
                    if j == 0:
                        P.add("dve", lambda e: e.tensor_copy(out=SbG[g][0], in_=spG[g]), reads=[(f"spG{g}",)], writes=[(f"SbG{g}0",)])
                    else:
                        so = SbG[g][(j - 1) % 2]
                        P.add("dve", lambda e: e.tensor_tensor(out=SbG[g][j % 2], in0=so, in1=spG[g], op=ALU.add),
                              reads=[(f"SbG{g}{(j - 1) % 2}",), (f"spG{g}",)], writes=[(f"SbG{g}{j % 2}",)])

                def Xg(j, g):
                    P.add("act", lambda e: e.activation(out=aG[g], in_=PGv(j, g), func=AF.Exp),
                          reads=[("ps", PGb(j, g)[0]), ("ps", PGb(j, g)[1])], writes=[(f"aG{g}",)])

                def AVg(j, g, kt=kt, hq=hq, nst=nst, obase=obase):
                    kb = 8 * kt + 7 - j
                    hp = 2 * hq + g
                    ob = obase + g
                    for c in range(2):
                        h = 2 * hp + c
                        tp = (0, 64) if c == 1 else None
                        P.add("pe", lambda e, c=c, h=h, tp=tp: e.matmul(bank(ob)[64 * c:64 * c + 64, :], lhsT=V[:, kb, h * 64:(h + 1) * 64],
                                                                        rhs=aG[g][:, c * 512:(c + 1) * 512],
                                                                        start=(j == 0), stop=(j == nst - 1), tile_position=tp),
                              reads=[("V", kb), (f"aG{g}",)], writes=[("ps", ob, c)])

                Zg(0, 0); Zg(0, 1)
                Eg(0, 0); Eg(0, 1); Lg(0, 0); Lg(0, 1)
                for j in range(nst):
                    TRg(j, 0); SUg(j, 0)
                    TRg(j, 1); SUg(j, 1)
                    if j + 1 < nst:
                        Zg(j + 1, 0)
                    Xg(j, 0)
                    AVg(j, 0)
                    Xg(j, 1)
                    if j + 1 < nst:
                        Zg(j + 1, 1)
                    AVg(j, 1)
                    if j + 1 < nst:
                        Eg(j + 1, 0); NOg(j + 1, 0)
                        Eg(j + 1, 1); NOg(j + 1, 1)
                        Lg(j + 1, 0); Lg(j + 1, 1)
                for g in range(2):
                    hp = 2 * hq + g
                    ob = obase + g
                    P.add("dve", lambda e, ob=ob, hp=hp, kt=kt: e.tensor_copy(out=oT[:, hp, kt * 512:(kt + 1) * 512], in_=bank(ob)),
                          reads=[("ps", ob, 0), ("ps", ob, 1)], writes=[("oT", hp, kt)])

    BIG = 1.0e4
    if stage >= 4:
        AR.release("KT", "V", "QT", "mkb")
        AR.release("eG0", "eG1", "spG0", "spG1", "aG0", "aG1", "SbG00", "SbG01", "SbG10", "SbG11")
        h2T = sb("h2T", [128, 8, TOK_OWN], BF16)
        gatesT = sb("gatesT", [64, TOK_OWN], BF16)
        wgab_t = [sb(f"wgab{i}", [128, 8, 256], BF16) for i in range(4)]
        hTm_t = [sb(f"hTm{i}", [128, 8, 512], BF16) for i in range(2)]
        siga_t = [sb(f"siga{i}", [128, 512], F32) for i in range(2)]
        sigb_t = [sb(f"sigb{i}", [128, 512], F32) for i in range(2)]
        m1_t = [sb(f"m1{i}", [128, 512], F32) for i in range(2)]
        m2_t = [sb(f"m2{i}", [128, 512], F32) for i in range(2)]
        merged = sb("merged", [128, 8, 512], BF16)
        xo_t = [sb(f"xo{i}", [128, D], F32) for i in range(3)]
        xn_t[0] = sb("xn0b", [128, D], BF16); xn_t[1] = sb("xn1b", [128, D], BF16)
        junk4 = sb("junkb", [128, D], BF16)
        rt = {nm: sb("rt_" + nm, [128, 64], F32) for nm in ("sc", "sel", "g8", "selm", "emask", "w")}
        rs8 = {nm: sb("rs_" + nm, [128, 8], F32) for nm in ("grp", "gs", "gmask", "pen", "top8")}
        rs1 = {nm: sb("r1_" + nm, [128, 1], F32) for nm in ("wsum", "rws")}
        gates_bf = sb("gates_bf", [128, 64], BF16)

        def rms2(xs, kx, nrows):
            i2 = cnt["xn"]; cnt["xn"] += 1
            xn = xn_t[i2 % 2]; kn = ("xn%db" % (i2 % 2),)
            s_ = cnt["st"] % 16; cnt["st"] += 1
            ks = ("stat", s_)
            P.add("act", lambda e: e.activation(out=junk4, in_=xs, func=AF.Square, accum_out=stat[:, 4 * s_:4 * s_ + 1]),
                  reads=[kx], writes=["junkb", ks])
            P.add("pool", lambda e: e.tensor_tensor(out=stat[:, 4 * s_ + 1:4 * s_ + 2], in0=stat[:, 4 * s_:4 * s_ + 1],
                                                    in1=deps_col, op=ALU.add),
                  reads=[ks, "deps"], writes=[ks])
            P.add("pool", lambda e: e.tensor_tensor(out=stat[:, 4 * s_ + 2:4 * s_ + 3], in0=stat[:, 4 * s_ + 1:4 * s_ + 2],
                                                    in1=mhalf_col, op=ALU.pow),
                  reads=[ks, "mhalf"], writes=[ks])
            P.add("dve", lambda e: e.tensor_scalar(out=xn, in0=xs, scalar1=stat[:, 4 * s_ + 2:4 * s_ + 3], scalar2=32.0,
                                                   op0=ALU.mult, op1=ALU.mult),
                  reads=[kx, ks], writes=[kn])
            return xn, kn

        merged_t = [merged, sb("merged1", [128, 8, 512], BF16)]
        cnt4 = {"ci": 0, "xi": 0}

        def load_hTm(kt):
            hTm = hTm_t[kt % 2]; khm = ("hTm%d" % (kt % 2),)
            dma("sp", hTm.rearrange("p a b -> p (a b)"), hT_scr[kt], reads=[("hT_scr", kt)], writes=[khm])

        chunk_dma_done = {}

        def chunk4_dma(kt, c, ci):
            wg_ = wgab_t[ci % 4]; kwg = ("wgab%d" % (ci % 4),)
            dma("pool", wg_[:, :, 0:128], win_v[:, :, 2048 + c * 128:2048 + (c + 1) * 128], writes=[kwg])
            dma("pool", wg_[:, :, 128:256], win_v[:, :, 3072 + c * 128:3072 + (c + 1) * 128], writes=[kwg])
            chunk_dma_done[(kt, c)] = ci

        def chunk4(kt, c):
            hTm = hTm_t[kt % 2]; khm = ("hTm%d" % (kt % 2),)
            mg = merged_t[kt % 2]; kmg = "merged" if kt % 2 == 0 else "merged1"
            ci = cnt4["ci"]; cnt4["ci"] += 1
            wg_ = wgab_t[ci % 4]; kwg = ("wgab%d" % (ci % 4),)
            sa_ = siga_t[ci % 2]; ksa = ("siga%d" % (ci % 2),)
            sb_ = sigb_t[ci % 2]; ksb = ("sigb%d" % (ci % 2),)
            m1 = m1_t[ci % 2]; km1 = ("m1%d" % (ci % 2),)
            m2 = m2_t[ci % 2]; km2 = ("m2%d" % (ci % 2),)
            if (kt, c) not in chunk_dma_done:
                chunk4_dma(kt, c, ci)
            for k in range(8):
                P.add("pe", lambda e, k=k: e.matmul(bank(0), lhsT=wg_[:, k, 0:128], rhs=hTm[:, k, :], start=(k == 0), stop=(k == 7)),
                      reads=[kwg, khm], writes=[("ps", 0)])
            for k in range(8):
                P.add("pe", lambda e, k=k: e.matmul(bank(1), lhsT=wg_[:, k, 128:256], rhs=hTm[:, k, :], start=(k == 0), stop=(k == 7)),
                      reads=[kwg, khm], writes=[("ps", 1)])
            P.add("act", lambda e: e.activation(out=sa_, in_=bank(0), func=AF.Sigmoid), reads=[("ps", 0)], writes=[ksa])
            P.add("act", lambda e: e.activation(out=sb_, in_=bank(1), func=AF.Sigmoid), reads=[("ps", 1)], writes=[ksb])
            for hp in range(4):
                P.add("pe", lambda e, hp=hp: e.matmul(bank(2), lhsT=wsb[:, hp, c * 128:(c + 1) * 128],
                                                      rhs=oT[:, hp, kt * 512:(kt + 1) * 512], start=(hp == 0), stop=(hp == 3)),
                      reads=["wsb", ("oT", hp, kt)], writes=[("ps", 2)])
            for g in range(4):
                P.add("pe", lambda e, g=g: e.matmul(bank(3), lhsT=wpo[:, g, c * 128:(c + 1) * 128],
                                                    rhs=pooledT[:, g, kt * 512:(kt + 1) * 512], start=(g == 0), stop=(g == 3)),
                      reads=["wpo", ("pooledT", g, kt)], writes=[("ps", 3)])
            P.add("dve", lambda e: e.tensor_tensor(out=m1, in0=bank(2), in1=sa_, op=ALU.mult), reads=[("ps", 2), ksa], writes=[km1])
            P.add("dve", lambda e: e.tensor_tensor(out=m2, in0=bank(3), in1=sb_, op=ALU.mult), reads=[("ps", 3), ksb], writes=[km2])
            P.add("dve", lambda e: e.tensor_tensor(out=mg[:, c, :], in0=m1, in1=m2, op=ALU.add), reads=[km1, km2], writes=[(kmg, c)])

        def block4_A(kt, b4):
            mg = merged_t[kt % 2]; kmg = "merged" if kt % 2 == 0 else "merged1"
            blk_i = kt * 4 + b4
            xi = cnt4["xi"]; cnt4["xi"] += 1
            xo = xo_t[xi % 3]; kxo = ("xo%d" % (xi % 3),)
            r0 = blk_i * 128
            dma("sp", xo, x_own[r0:r0 + 128, :], writes=[kxo])
            for hf in range(2):
                for c in range(8):
                    P.add("pe", lambda e, c=c, hf=hf: e.matmul(bank(4 + hf), lhsT=mg[:, c, b4 * 128:(b4 + 1) * 128],
                                                               rhs=wout[:, c, hf * 512:(hf + 1) * 512], start=(c == 0), stop=(c == 7)),
                          reads=[(kmg, c), "wout"], writes=[("ps", 4 + hf)])
            P.add("dve", lambda e: e.tensor_tensor(out=xo, in0=psum[:, 4 * 512:6 * 512], in1=xo, op=ALU.add),
                  reads=[("ps", 4), ("ps", 5), kxo], writes=[kxo])
            dma("sp", xnew_scr[r0:r0 + 128, :], xo, reads=[kxo], writes=[("xnew_scr", blk_i)])
            return rms2(xo, kxo, 128)

        def block4_B(kt, b4, xn, kn):
            blk_i = kt * 4 + b4
            transpose_mod(xn, kn, 128, h2T, ("h2T", blk_i), blk_i * 128, A2, "A2", B2, B2keys, (6,))

        def router4_A(kt, b4):
            blk_i = kt * 4 + b4
            lg = bank(7)[:, 0:64]
            for c in range(8):
                P.add("pe", lambda e, c=c: e.matmul(lg, lhsT=h2T[:, c, blk_i * 128:(blk_i + 1) * 128], rhs=wr[:, c, :],
                                                    start=(c == 0), stop=(c == 7)),
                      reads=[("h2T", blk_i), "wr"], writes=[("ps", 7)])
            sc, sel_, g8, selm, emask, w_ = (rt[n] for n in ("sc", "sel", "g8", "selm", "emask", "w"))
            grp, gs, gmask, pen, top8 = (rs8[n] for n in ("grp", "gs", "gmask", "pen", "top8"))
            wsum, rws = rs1["wsum"], rs1["rws"]
            P.add("act", lambda e: e.activation(out=sc, in_=lg, func=AF.Sigmoid), reads=[("ps", 7)], writes=["rt_sc"])
            P.add("dve", lambda e: e.tensor_tensor(out=sel_, in0=sc, in1=rbias, op=ALU.add), reads=["rt_sc", "rbias"], writes=["rt_sel"])
            for g in range(8):
                P.add("dve", lambda e, g=g: e.max(out=g8[:, g * 8:(g + 1) * 8], in_=sel_[:, g * 8:(g + 1) * 8]),
                      reads=["rt_sel"], writes=["rt_g8"])
            g8v = g8.rearrange("p (g e) -> p g e", e=8)
            P.add("dve", lambda e: e.tensor_tensor(out=grp, in0=g8v[:, :, 0], in1=g8v[:, :, 1], op=ALU.add),
                  reads=["rt_g8"], writes=["rs_grp"])
            P.add("dve", lambda e: e.max(out=gs, in_=grp), reads=["rs_grp"], writes=["rs_gs"])
            P.add("dve", lambda e: e.tensor_scalar(out=gmask, in0=grp, scalar1=gs[:, 3:4], scalar2=None, op0=ALU.is_ge),
                  reads=["rs_grp", "rs_gs"], writes=["rs_gmask"])
            P.add("dve", lambda e: e.tensor_scalar(out=pen, in0=gmask, scalar1=BIG, scalar2=-BIG, op0=ALU.mult, op1=ALU.add),
                  reads=["rs_gmask"], writes=["rs_pen"])
            P.add("dve", lambda e: e.tensor_tensor(out=selm.rearrange("p (g e) -> p g e", e=8), in0=sel_.rearrange("p (g e) -> p g e", e=8),
                                                   in1=pen.unsqueeze(2).to_broadcast([128, 8, 8]), op=ALU.add),
                  reads=["rt_sel", "rs_pen"], writes=["rt_selm"])
            P.add("dve", lambda e: e.max(out=top8, in_=selm), reads=["rt_selm"], writes=["rs_top8"])
            P.add("dve", lambda e: e.tensor_scalar(out=emask, in0=selm, scalar1=top8[:, 7:8], scalar2=None, op0=ALU.is_ge),
                  reads=["rt_selm", "rs_top8"], writes=["rt_emask"])
            P.add("dve", lambda e: e.tensor_tensor(out=w_, in0=sc, in1=emask, op=ALU.mult), reads=["rt_sc", "rt_emask"], writes=["rt_w"])
            P.add("dve", lambda e: e.reduce_sum(out=wsum, in_=w_, axis=mybir.AxisListType.X), reads=["rt_w"], writes=["r1_wsum"])
            P.add("dve", lambda e: e.reciprocal(out=rws, in_=wsum), reads=["r1_wsum"], writes=["r1_rws"])
            P.add("dve", lambda e: e.tensor_scalar(out=gates_bf, in0=w_, scalar1=rws[:, 0:1], scalar2=2.5, op0=ALU.mult, op1=ALU.mult),
                  reads=["rt_w", "r1_rws"], writes=["gates_bf"])

        def router4_B(kt, b4):
            blk_i = kt * 4 + b4
            gtp = bank_bf(7)[0:64, 256:384]
            P.add("pe", lambda e: e.transpose(out=gtp, in_=gates_bf, identity=ident), reads=["gates_bf", "ident"], writes=[("ps", 7)])
            P.add("act", lambda e: e.activation(out=gatesT[:, blk_i * 128:(blk_i + 1) * 128], in_=gtp, func=AF.Copy),
                  reads=[("ps", 7)], writes=[("gatesT", blk_i)])

        load_hTm(0)
        for c in range(8):
            chunk4(0, c)
        prev = None
        pend_rb = None
        defer_r = []
        for kt in range(NT_OWN):
            nxt = kt + 1 < NT_OWN
            if nxt:
                load_hTm(kt + 1)
            elif stage >= 5:
                AR.release("wgab0", "wgab1", "wgab2", "wgab3", "hTm0", "hTm1", "siga0", "siga1", "sigb0", "sigb1",
                           "m10", "m11", "m20", "m21")
                wg_e = sb("wg0", [128, 4, 8, 128], BF16)
                wu_e = sb("wu0", [128, 4, 8, 128], BF16)
                wd_e = sb("wd0", [128, 4, D], BF16)
                dma("pool", wg_e, w_exp_gate[0:4].rearrange("e (k p) f -> p e k f", p=128), writes=[("wg0",)])
                dma("pool", wu_e, w_exp_up[0:4].rearrange("e (k p) f -> p e k f", p=128), writes=[("wu0",)])
                dma("pool", wd_e, w_exp_down[0:4].rearrange("e f d -> f e d"), writes=[("wd0",)])
            for b4 in range(4):
                if nxt:
                    chunk4_dma(kt + 1, 2 * b4, cnt4["ci"])
                    chunk4_dma(kt + 1, 2 * b4 + 1, cnt4["ci"] + 1)
                xn_kn = block4_A(kt, b4)
                if prev is not None:
                    block4_B(*prev)
                if nxt:
                    chunk4(kt + 1, 2 * b4)
                if pend_rb is not None:
                    router4_B(*pend_rb)
                    pend_rb = None
                if prev is not None:
                    if stage >= 5 and (prev[0], prev[1]) == (NT_OWN - 1, 2):
                        defer_r.append((prev[0], prev[1]))
                    else:
                        router4_A(prev[0], prev[1])
                        pend_rb = (prev[0], prev[1])
                if nxt:
                    chunk4(kt + 1, 2 * b4 + 1)
                prev = (kt, b4) + xn_kn
        block4_B(*prev)
        if pend_rb is not None:
            router4_B(*pend_rb)
        if stage >= 5:
            defer_r.append((prev[0], prev[1]))
        else:
            router4_A(prev[0], prev[1])
            router4_B(prev[0], prev[1])

        def gates_store(t_):
            dma("sp", gates_scr[:, t_ * 512:(t_ + 1) * 512], gatesT[:, t_ * 512:(t_ + 1) * 512],
                reads=[("gatesT", t_ * 4 + q) for q in range(4)], writes=[("gates_scr", t_)])
        for t_ in range(3 if stage >= 5 else 4):
            gates_store(t_)

    if stage >= 5:
        AR.release("oT", "pooledT", "wsb", "wpo", "wout",
                   "merged", "merged1", "xo0", "xo1", "xo2", "xn0b", "xn1b", "junkb")
        acc_t = [sb(f"acc{q}", [128, 4, D], F32) for q in range(4)]
        gbc_t = [sb(f"gbc{q}", [128, 512], BF16) for q in range(4)]
        gbc_n = {"n": 0}

        def accv(b_):
            return acc_t[b_ // 4][:, b_ % 4, :]

        def acck(b_):
            return (f"acc{b_ // 4}", b_ % 4)
        wg_t = [wg_e, sb("wg1", [128, 4, 8, 128], BF16)]
        wu_t = [wu_e, sb("wu1", [128, 4, 8, 128], BF16)]
        wd_t = [wd_e, sb("wd1", [128, 4, D], BF16)]
        sa_t = [sb(f"sa{i}", [128, 512], F32) for i in range(2)]
        t1_t = [sb(f"t1{i}", [128, 512], F32) for i in range(2)]
        hm_t = [sb(f"hm{i}", [128, 4, 512], BF16) for i in range(2)]
        xf_t = [sb(f"xf{i}", [128, D], F32) for i in range(2)]
        n_grp = 17
        ei = 0
        ti = 0
        yi = {"n": 0}
        GBK = 0
        UBK = (1, 2)
        SBK = 3
        YBK = ((4, 5), (6, 7))

        def GU(grp_i, t, ne, wg_, wu_, kw, kwu, hm, khm_):
            nonlocal ei
            for el in range(ne):
                ab = ei % 2
                sa_ = sa_t[ab]; ksa = ("sa%d" % ab,)
                t1 = t1_t[ab]; kt1 = ("t1%d" % ab,)
                ub = UBK[ab]
                ei += 1
                for k in range(8):
                    P.add("pe", lambda e, k=k, el=el, wg_=wg_, t=t: e.matmul(bank(GBK), lhsT=wg_[:, el, k, :],
                                                                            rhs=h2T[:, k, t * 512:(t + 1) * 512], start=(k == 0), stop=(k == 7)),
                          reads=[kw] + [("h2T", t * 4 + q) for q in range(4)], writes=[("ps", GBK)])
                for k in range(8):
                    P.add("pe", lambda e, k=k, el=el, ub=ub, wu_=wu_, t=t: e.matmul(bank(ub), lhsT=wu_[:, el, k, :],
                                                                                   rhs=h2T[:, k, t * 512:(t + 1) * 512], start=(k == 0), stop=(k == 7)),
                          reads=[kwu] + [("h2T", t * 4 + q) for q in range(4)], writes=[("ps", ub)])
                P.add("act", lambda e, sa_=sa_: e.activation(out=sa_, in_=bank(GBK), func=AF.Silu), reads=[("ps", GBK)], writes=[ksa])
                if grp_i < 16:
                    eg = grp_i * 4 + el
                    gi_ = gbc_n["n"] % 4; gbc_n["n"] += 1
                    gb = gbc_t[gi_]; kgb = ("gbc%d" % gi_,)
                    dma("sp", gb, gates_scr[eg:eg + 1, t * 512:(t + 1) * 512].to_broadcast([128, 512]),
                        reads=[("gates_scr", t)], writes=[kgb])
                    P.add("dve", lambda e, t1=t1, sa_=sa_, ub=ub: e.tensor_tensor(out=t1, in0=bank(ub), in1=sa_, op=ALU.mult),
                          reads=[("ps", ub), ksa], writes=[kt1])
                    P.add("dve", lambda e, t1=t1, hm=hm, el=el, gb=gb: e.tensor_tensor(out=hm[:, el, :], in0=gb, in1=t1, op=ALU.mult),
                          reads=[kgb, kt1], writes=[(khm_, el)])
                else:
                    P.add("dve", lambda e, sa_=sa_, hm=hm, el=el, ub=ub: e.tensor_tensor(out=hm[:, el, :], in0=bank(ub), in1=sa_, op=ALU.mult),
                          reads=[("ps", ub), ksa], writes=[(khm_, el)])

        def DN(grp_i, t, ne, wd_, kwd, hm, khm_):
            for b4 in range(4):
                blk_i = t * 4 + b4
                yb = YBK[yi["n"] % 2]; yi["n"] += 1
                ybank = psum[:, yb[0] * 512:(yb[1] + 1) * 512]
                for el in range(ne):
                    for hf in range(2):
                        P.add("pe", lambda e, el=el, hf=hf, b4=b4, hm=hm, wd_=wd_, ne=ne, yb=yb: e.matmul(
                            bank(yb[hf]), lhsT=hm[:, el, b4 * 128:(b4 + 1) * 128], rhs=wd_[:, el, hf * 512:(hf + 1) * 512],
                            start=(el == 0), stop=(el == ne - 1)),
                              reads=[(khm_, el), kwd], writes=[("ps", yb[hf])])
                r0 = blk_i * 128
                if grp_i == 0:
                    xf = xf_t[blk_i % 2]; kxf = ("xf%d" % (blk_i % 2),)
                    dma("sp", xf, xnew_scr[r0:r0 + 128, :], reads=[("xnew_scr", blk_i)], writes=[kxf])
                    P.add("dve", lambda e, blk_i=blk_i, ybank=ybank, xf=xf: e.tensor_tensor(out=accv(blk_i), in0=ybank, in1=xf, op=ALU.add),
                          reads=[("ps", yb[0]), ("ps", yb[1]), kxf], writes=[acck(blk_i)])
                else:
                    P.add("dve", lambda e, blk_i=blk_i, ybank=ybank: e.tensor_tensor(out=accv(blk_i), in0=ybank, in1=accv(blk_i), op=ALU.add),
                          reads=[("ps", yb[0]), ("ps", yb[1]), acck(blk_i)], writes=[acck(blk_i)])
                if grp_i == n_grp - 1:
                    final_ops.append(dma("sp", out[r0:r0 + 128, :], accv(blk_i), reads=[acck(blk_i)]))

        pend = None
        for grp_i in range(n_grp):
            sl = grp_i % 2
            wg_, wu_, wd_ = wg_t[sl], wu_t[sl], wd_t[sl]
            kw = ("wg%d" % sl,); kwu = ("wu%d" % sl,); kwd = ("wd%d" % sl,)
            if grp_i == 0:
                ne = 4
            elif grp_i < 16:
                ne = 4
                e0 = grp_i * 4
                dma("pool", wg_, w_exp_gate[e0:e0 + 4].rearrange("e (k p) f -> p e k f", p=128), writes=[kw])
                dma("pool", wu_, w_exp_up[e0:e0 + 4].rearrange("e (k p) f -> p e k f", p=128), writes=[kwu])
                dma("pool", wd_, w_exp_down[e0:e0 + 4].rearrange("e f d -> f e d"), writes=[kwd])
            else:
                ne = 2
                dma("pool", wg_[:, 0:2], w_sh_gate.rearrange("(k p) (e f) -> p e k f", p=128, f=128), writes=[kw])
                dma("pool", wu_[:, 0:2], w_sh_up.rearrange("(k p) (e f) -> p e k f", p=128, f=128), writes=[kwu])
                dma("pool", wd_[:, 0:2], w_sh_down.rearrange("(e f) d -> f e d", f=128), writes=[kwd])
            for el in range(ne):
                P.add("pool", lambda e, el=el, wd_=wd_: e.tensor_tensor(out=wd_[:, el, :], in0=wd_[:, el, :], in1=gate2_bc, op=ALU.mult),
                      reads=[kwd, "gate2_bc"], writes=[kwd])
            for t in range(4):
                hm = hm_t[ti % 2]; khm_ = "hm%d" % (ti % 2)
                ti += 1
                GU(grp_i, t, ne, wg_, wu_, kw, kwu, hm, khm_)
                if pend is not None:
                    DN(*pend)
                pend = (grp_i, t, ne, wd_, kwd, hm, khm_)
                if grp_i == 0 and len(defer_r) == 2:
                    if t == 0:
                        router4_A(*defer_r[0])
                    elif t == 1:
                        router4_B(*defer_r[0])
                        router4_A(*defer_r[1])
                    elif t == 2:
                        router4_B(*defer_r[1])
                        gates_store(3)
        DN(*pend)

    if dbg and stage == 4:
        AR.release("oT", "pooledT", "wout")
        dtmp = sb("dtmp", [128, 4096], F32)
        P.add("dve", lambda e: e.tensor_copy(out=dtmp[:, 0:2048], in_=h2T[:, 0, :]),
              reads=[("h2T", b_) for b_ in range(16)], writes=["dtmp"])
        P.add("dve", lambda e: e.tensor_copy(out=dtmp[0:64, 2048:4096], in_=gatesT),
              reads=[("gatesT", b_) for b_ in range(16)], writes=["dtmp"])
        final_ops.append(dma("sp", dbg_out[:, 0:4096], dtmp, reads=["dtmp"]))
        final_ops.append(dma("sp", out, xnew_scr, reads=[("xnew_scr", b_) for b_ in range(16)]))

    if dbg and stage in (2, 3):
        if stage == 2:
            AR.release("wqkvu", "KT", "V")
        else:
            AR.release("KT", "V")
        dtmp = sb("dtmp", [128, 8192], F32)
        P.add("dve", lambda e: e.tensor_copy(out=dtmp[:, 0:2048], in_=QT[:, 0, :]),
              reads=[("QT", 0, kt) for kt in range(4)], writes=["dtmp"])
        P.add("dve", lambda e: e.tensor_copy(out=dtmp[:, 2048:4096], in_=pooledT[:, 0, :]),
              reads=[("pooledT", 0, kt) for kt in range(4)], writes=["dtmp"])
        P.add("dve", lambda e: e.tensor_copy(out=dtmp[:, 4096:6144], in_=pooledT[:, 3, :]),
              reads=[("pooledT", 3, kt) for kt in range(4)], writes=["dtmp"])
        if stage == 3:
            P.add("dve", lambda e: e.tensor_copy(out=dtmp[:, 6144:7168], in_=oT[:, 0, 0:1024]),
                  reads=[("oT", 0, kt) for kt in range(2)], writes=["dtmp"])
            P.add("dve", lambda e: e.tensor_copy(out=dtmp[:, 7168:8192], in_=oT[:, 3, 0:1024]),
                  reads=[("oT", 3, kt) for kt in range(2)], writes=["dtmp"])
        final_ops.append(dma("sp", dbg_out[:, 0:8192], dtmp, reads=["dtmp"]))

    if dbg and stage == 1:
        AR.release("wqkvu")
        dtmp = sb("dtmp", [128, 8192], F32)
        P.add("dve", lambda e: e.tensor_copy(out=dtmp[:, 0:4096], in_=KT[:, 0, :]),
              reads=[("KT", 0, T) for T in range(8)], writes=["dtmp"])
        P.add("dve", lambda e: e.tensor_copy(out=dtmp[:, 4096:8192], in_=V[:, 0:8, :].rearrange("p a b -> p (a b)")),
              reads=[("V", i) for i in range(8)], writes=["dtmp"])
        final_ops.append(dma("sp", dbg_out[:, 0:8192], dtmp, reads=["dtmp"]))
        dt2 = sb("dt2", [128, 64], F32)
        P.add("dve", lambda e: e.tensor_copy(out=dt2[:, 0:48], in_=modT), reads=[("modT", j) for j in range(16)], writes=["dt2"])
        final_ops.append(dma("sp", dbg_out[:, 8192:8192 + 48], dt2[:, 0:48], reads=["dt2"]))

    P.emit(nc, st, final_ops)
    P.stats["arena_peak"] = AR.peak
    st.close()
    return nc, P


def make_consts():
    ident = np.eye(128, dtype=np.float32)
    j = np.arange(128)[:, None]
    k = np.arange(128)[None, :]
    ntri = np.where(j >= k, -1.0, 0.0).astype(np.float32)
    blk = ((j // 64) == (k // 64)).astype(np.float32)
    sel = np.zeros((64, 64, 128), np.float32)
    sel[np.arange(64), np.arange(64), :] = 1.0
    return ident, ntri, blk, sel.reshape(64, 64 * 128)


def make_maskb(half):
    m = np.zeros((128, 8, 512), np.float32)
    p = np.arange(128)[:, None]
    f = np.arange(512)[None, :]
    for jj in range(8):
        kb = 7 - jj
        kpos = kb * 128 + p
        qpos = half * 512 + f
        m[:, jj, :] = np.where(kpos < qpos, 0.0, NEG)
    return m.reshape(128, 8 * 512)


def colT(v, n):
    return np.ascontiguousarray(np.asarray(v, np.float32).reshape(n, 128).T)


def make_in_maps(inp):
    ident, ntri, blk, sel = make_consts()
    maps = []
    x = np.asarray(inp["x"], np.float32)
    f = lambda k: np.asarray(inp[k], np.float32)[0]
    for core in range(NCORES):
        b, half = core // 2, core % 2
        tiles = [2 * k + half for k in range(4)]
        x_own = np.concatenate([x[b, t * 512:(t + 1) * 512] for t in tiles], axis=0)
        x_halo = np.zeros((64, D), np.float32)
        halo_mask = np.zeros((128, 64), np.float32)
        invcnt = np.zeros((128, 4, 16), np.float32)
        for k, t in enumerate(tiles):
            if t > 0:
                x_halo[k * 16:(k + 1) * 16] = x[b, t * 512 - 16:t * 512]
                halo_mask[:, k * 16:(k + 1) * 16] = 1.0
        for g, w in enumerate((2, 4, 8, 16)):
            if tiles[0] == 0:
                invcnt[:, g, :] = 1.0 / np.minimum(np.arange(16) + 1, w)[None, :]
            else:
                invcnt[:, g, :] = 1.0 / w
        m = {
            "x_all": np.ascontiguousarray(x[b]),
            "x_own": np.ascontiguousarray(x_own),
            "x_halo": x_halo,
            "halo_mask": halo_mask,
            "invcnt": invcnt.reshape(128, 64),
            "maskb": make_maskb(half),
            "c_in": colT(np.asarray(inp["c"], np.float32)[b], 8),
            "w_ada": f("w_ada"),
            "b_ada": f("b_ada").reshape(1, -1),
            "b_adaT": colT(f("b_ada"), 48),
            "g_norm1T": colT(f("g_norm1"), 8),
            "w_in": f("w_in"),
            "g_q2": np.ascontiguousarray(np.tile(f("g_q"), 2).reshape(128, 1)),
            "g_k2": np.ascontiguousarray(np.tile(f("g_k"), 2).reshape(128, 1)),
            "w_sb_out": f("w_sb_out"),
            "w_pool_group": f("w_pool_group"),
            "pool_scaleT": colT(f("pool_scale"), 4),
            "w_pool_out": f("w_pool_out"),
            "w_out": f("w_out"),
            "g_norm2T": colT(f("g_norm2"), 8),
            "w_router": f("w_router"),
            "router_bias": f("router_bias").reshape(1, -1),
            "w_exp_gate": f("w_exp_gate"),
            "w_exp_up": f("w_exp_up"),
            "w_exp_down": f("w_exp_down"),
            "w_sh_gate": f("w_sh_gate"),
            "w_sh_up": f("w_sh_up"),
            "w_sh_down": f("w_sh_down"),
            "c_ident": ident, "c_ntri": ntri, "c_blk": blk, "c_sel": sel,
        }
        maps.append(m)
    return maps


def kernel(**inputs):
    nc, _ = build()
    in_maps = make_in_maps(inputs)
    res = run_bass_kernel_spmd(nc, in_maps, core_ids=list(range(NCORES)))
    outp = np.zeros((4, S, D), np.float32)
    for core in range(NCORES):
        b, half = core // 2, core % 2
        o = np.asarray(res.results[core]["out"]).reshape(TOK_OWN, D)
        for k in range(4):
            t = 2 * k + half
            outp[b, t * 512:(t + 1) * 512] = o[k * 512:(k + 1) * 512]
    return outp
```

```python
import numpy as np
from contextlib import ExitStack

import concourse.bass as bass
import concourse.mybir as mybir
from concourse.bass_utils import run_bass_kernel_spmd

F32 = mybir.dt.float32
BF16 = mybir.dt.bfloat16
AF = mybir.ActivationFunctionType
ALU = mybir.AluOpType

NCORES = 8
S = 4096
D = 1024
NT_OWN = 4
TOK_OWN = 2048
EPS = 1e-6
NEG = -30000.0
ARENA_BYTES = 206 * 1024
NDUM = 0


def kname(key):
    return key[0] if isinstance(key, tuple) else key


class Op:
    __slots__ = ("eng", "fn", "reads", "writes", "dma", "deps", "sig", "need", "idx")

    def __init__(self, eng, fn, reads, writes, dma):
        self.eng = eng
        self.fn = fn
        self.reads = tuple(reads)
        self.writes = tuple(writes)
        self.dma = dma
        self.deps = ()
        self.sig = None
        self.need = False


class Prog:
    ENGS = ("pe", "act", "dve", "pool", "sp")
    N_DMA_SEM = {"sp": 40, "pool": 24, "act": 8}

    def __init__(self):
        self.ops = []
        self.alias = {}
        self.unwritten = set()

    def add(self, eng, fn, reads=(), writes=(), dma=False):
        op = Op(eng, fn, reads, writes, dma)
        op.idx = len(self.ops)
        self.ops.append(op)
        return op

    def emit(self, nc, stack, final_ops):
        ops = self.ops
        last_w = {}
        readers = {}
        touch = {}
        touch_dma = {}
        synced = set()
        for i, op in enumerate(ops):
            deps = set()
            for r in op.reads:
                if r in last_w:
                    deps.add(last_w[r])
                else:
                    self.unwritten.add(r)
            for w in op.writes:
                if w in last_w:
                    deps.add(last_w[w])
                for rd in readers.get(w, ()):
                    deps.add(rd)
            for key in op.reads + op.writes:
                nm = kname(key)
                if nm in self.alias and (op.eng, nm) not in synced:
                    synced.add((op.eng, nm))
                    for old in self.alias[nm]:
                        for _, oi in touch.get(old, {}).items():
                            deps.add(oi)
                        for oi in touch_dma.get(old, ()):
                            deps.add(oi)
            deps.discard(i)
            op.deps = deps
            for r in op.reads:
                readers.setdefault(r, []).append(i)
            for w in op.writes:
                last_w[w] = i
                readers[w] = []
            for key in op.reads + op.writes:
                nm = kname(key)
                if op.dma:
                    touch_dma.setdefault(nm, []).append(i)
                else:
                    touch.setdefault(nm, {})[op.eng] = i
        dma_slots = {q: [None] * n for q, n in self.N_DMA_SEM.items()}
        dma_ctr = {q: 0 for q in self.N_DMA_SEM}
        dma_slot_of = {}
        for i, op in enumerate(ops):
            if op.dma:
                q = op.eng
                s = dma_ctr[q] % self.N_DMA_SEM[q]
                dma_ctr[q] += 1
                prev = dma_slots[q][s]
                if prev is not None:
                    op.deps.add(prev)
                dma_slots[q][s] = i
                dma_slot_of[i] = s
        fin = self.add("sp", None, (), ())
        fin.deps = set(o.idx for o in final_ops)
        for op in ops:
            nd = set()
            best = {}
            for d in op.deps:
                dop = ops[d]
                if dop.dma:
                    nd.add(d)
                    continue
                if dop.eng == op.eng and op.eng == "pe" and not op.dma:
                    continue
                if dop.eng not in best or best[dop.eng] < d:
                    best[dop.eng] = d
            nd.update(best.values())
            for d in nd:
                ops[d].need = True
            op.deps = nd
        eng_sem = {e: stack.enter_context(nc.semaphore("sem_" + e)) for e in self.ENGS}
        dma_sem = {q: [stack.enter_context(nc.semaphore(f"dsem_{q}{j}")) for j in range(n)]
                   for q, n in self.N_DMA_SEM.items()}
        eng_cnt = {e: 0 for e in self.ENGS}
        dma_use = {q: [0] * n for q, n in self.N_DMA_SEM.items()}
        for i, op in enumerate(ops):
            if op.dma:
                s = dma_slot_of[i]
                dma_use[op.eng][s] += 1
                op.sig = (dma_sem[op.eng][s], 16 * dma_use[op.eng][s], 16)
            elif op.need:
                eng_cnt[op.eng] += 1
                op.sig = (eng_sem[op.eng], eng_cnt[op.eng], 1)
        per_eng = {e: [op for op in ops if op.eng == e] for e in self.ENGS}
        self.stats = {e: len(per_eng[e]) for e in self.ENGS}
        self.stats["sig"] = dict(eng_cnt)
        self.stats["unwritten"] = sorted(str(u) for u in self.unwritten)

        def run(ename, eng):
            waited = {}
            nwait = 0
            for op in per_eng[ename]:
                need = {}
                for d in op.deps:
                    sem, val, _ = ops[d].sig
                    k = id(sem)
                    if waited.get(k, 0) >= val:
                        continue
                    if k not in need or need[k][1] < val:
                        need[k] = (sem, val)
                for k, (sem, val) in need.items():
                    eng.wait_ge(sem, val)
                    waited[k] = val
                    nwait += 1
                if op.fn is None:
                    continue
                inst = op.fn(eng)
                if op.sig is not None:
                    inst.then_inc(op.sig[0], op.sig[2])
            self.stats["wait_" + ename] = nwait

        with nc.Block() as block:
            @block.tensor
            def _(e):
                run("pe", e)

            @block.scalar
            def _(e):
                run("act", e)

            @block.vector
            def _(e):
                run("dve", e)

            @block.gpsimd
            def _(e):
                run("pool", e)

            @block.sync
            def _(e):
                run("sp", e)


class Arena:
    def __init__(self, prog, tensor, nbytes):
        self.P = prog
        self.t = tensor
        self.free = [(0, nbytes)]
        self.live = {}
        self.dead = []
        self.peak = 0

    def alloc(self, name, shape, dt):
        esz = 4 if dt == F32 else 2
        n = 1
        for s in shape[1:]:
            n *= s
        nbytes = (n * esz + 63) // 64 * 64
        for idx, (o, sz) in enumerate(self.free):
            if sz >= nbytes:
                off = o
                if sz == nbytes:
                    self.free.pop(idx)
                else:
                    self.free[idx] = (o + nbytes, sz - nbytes)
                break
        else:
            raise RuntimeError(f"arena full allocating {name} {nbytes}B; live={ {k: v[1] for k, v in self.live.items()} }")
        assert name not in self.live and name not in self.P.alias
        self.live[name] = (off, nbytes)
        self.peak = max(self.peak, off + nbytes)
        al = [d[0] for d in self.dead if d[1] < off + nbytes and off < d[1] + d[2]]
        self.P.alias[name] = al
        v = self.t[0:shape[0], off // 2:(off + n * esz) // 2]
        if dt == F32:
            v = v.bitcast(F32)
        if len(shape) == 3:
            v = v.rearrange("p (a b) -> p a b", b=shape[2])
        elif len(shape) == 4:
            v = v.rearrange("p (a b c) -> p a b c", b=shape[2], c=shape[3])
        return v

    def release(self, *names):
        for name in names:
            off, nbytes = self.live.pop(name)
            self.dead.append((name, off, nbytes))
            self.free.append((off, nbytes))
        self.free.sort()
        merged = []
        for o, sz in self.free:
            if merged and merged[-1][0] + merged[-1][1] == o:
                merged[-1] = (merged[-1][0], merged[-1][1] + sz)
            else:
                merged.append((o, sz))
        self.free = merged


def build(stage=99, dbg=False):
    nc = bass.Bass("TRN2", target_bir_lowering=False)
    P = Prog()
    st = ExitStack()

    def din(name, shape, dt=F32):
        return nc.dram_tensor(name, list(shape), dt, kind="ExternalInput").ap()

    x_all = din("x_all", [S, D])
    x_own = din("x_own", [TOK_OWN, D])
    x_halo = din("x_halo", [64, D])
    halo_mask = din("halo_mask", [128, 64])
    invcnt = din("invcnt", [128, 64])
    maskb = din("maskb", [128, 8 * 512])
    c_in = din("c_in", [128, 8])
    w_ada = din("w_ada", [D, 6 * D])
    b_ada = din("b_ada", [1, 6 * D])
    b_adaT = din("b_adaT", [128, 48])
    g_norm1 = din("g_norm1T", [128, 8])
    w_in = din("w_in", [D, 4096])
    g_q = din("g_q2", [128, 1])
    g_k = din("g_k2", [128, 1])
    w_sb_out = din("w_sb_out", [512, D])
    w_pool_group = din("w_pool_group", [4, 128, 128])
    pool_scale = din("pool_scaleT", [128, 4])
    w_pool_out = din("w_pool_out", [512, D])
    w_out = din("w_out", [D, D])
    g_norm2 = din("g_norm2T", [128, 8])
    w_router = din("w_router", [D, 64])
    router_bias = din("router_bias", [1, 64])
    w_exp_gate = din("w_exp_gate", [64, D, 128])
    w_exp_up = din("w_exp_up", [64, D, 128])
    w_exp_down = din("w_exp_down", [64, 128, D])
    w_sh_gate = din("w_sh_gate", [D, 256])
    w_sh_up = din("w_sh_up", [D, 256])
    w_sh_down = din("w_sh_down", [256, D])
    c_ident = din("c_ident", [128, 128])
    c_ntri = din("c_ntri", [128, 128])
    c_blk = din("c_blk", [128, 128])
    c_sel = din("c_sel", [64, 64 * 128])

    out = nc.dram_tensor("out", [TOK_OWN, D], F32, kind="ExternalOutput").ap()
    mod_scr = nc.dram_tensor("mod_scr", [1, 2 * D], F32, kind="Internal").ap()
    hT_scr = nc.dram_tensor("hT_scr", [NT_OWN, 128, 8 * 512], BF16, kind="Internal").ap()
    xnew_scr = nc.dram_tensor("xnew_scr", [TOK_OWN, D], F32, kind="Internal").ap()
    gates_scr = nc.dram_tensor("gates_scr", [64, TOK_OWN], BF16, kind="Internal").ap()
    dbg_out = None
    if dbg:
        dbg_out = nc.dram_tensor("dbg", [128, 16384], F32, kind="ExternalOutput").ap()

    arena_t = st.enter_context(nc.sbuf_tensor("arena", [128, ARENA_BYTES // 2], BF16))
    AR = Arena(P, arena_t, ARENA_BYTES)
    sb = AR.alloc

    psum = st.enter_context(nc.psum_tensor("psum", [128, 8 * 512], F32))

    def bank(i):
        return psum[:, i * 512:(i + 1) * 512]

    def bank_bf(i):
        return psum[:, i * 512:(i + 1) * 512].bitcast(BF16)

    final_ops = []

    def dma(q, out_ap, in_ap, reads=(), writes=()):
        return P.add(q, lambda e: e.dma_start(out=out_ap, in_=in_ap), reads=reads, writes=writes, dma=True)

    ident = sb("ident", [128, 128], BF16)
    ntri = sb("ntri", [128, 128], BF16)
    nones = sb("nones", [128, 128], BF16)
    blk = sb("blk", [128, 128], BF16)
    eps_col = sb("eps", [128, 1], F32)
    one_col = sb("one", [128, 1], F32)
    dma("pool", ident, c_ident, writes=["ident"])
    dma("pool", ntri, c_ntri, writes=["ntri"])
    dma("pool", blk, c_blk, writes=["blk"])
    P.add("dve", lambda e: e.memset(nones, -1.0), writes=["nones"])
    P.add("dve", lambda e: e.memset(eps_col, EPS), writes=["eps"])
    P.add("dve", lambda e: e.memset(one_col, 1.0), writes=["one"])
    deps_col = sb("deps", [128, 1], F32)
    mhalf_col = sb("mhalf", [128, 1], F32)
    P.add("dve", lambda e: e.memset(deps_col, D * EPS), writes=["deps"])
    P.add("dve", lambda e: e.memset(mhalf_col, -0.5), writes=["mhalf"])

    c_sb = sb("c_sb", [128, 8], F32)
    c_bf = sb("c_bf", [128, 8], BF16)
    modT = sb("modT", [128, 48], F32)
    bT = sb("bT", [128, 48], F32)
    g1T = sb("g1T", [128, 8], F32)
    g2T = sb("g2T", [128, 8], F32)
    A1 = sb("A1", [128, 8], F32)
    A2 = sb("A2", [128, 8], F32)
    gate1_bc = sb("gate1_bc", [128, D], F32)
    gate2_bc = sb("gate2_bc", [128, D], F32)
    dma("sp", c_sb, c_in, writes=["c_sb"])
    dma("sp", bT, b_adaT, writes=["bT"])
    dma("sp", g1T, g_norm1, writes=["g1T"])
    dma("sp", g2T, g_norm2, writes=["g2T"])
    P.add("act", lambda e: e.activation(out=c_bf, in_=c_sb, func=AF.Silu), reads=["c_sb"], writes=["c_bf"])
    wada_v = w_ada.rearrange("(k p) n -> p k n", p=128)
    wada_t = [sb(f"wada{i}", [128, 8, 512], BF16) for i in range(2)]
    brow_t = [sb(f"brow{i}", [1, 512], F32) for i in range(2)]
    mrow_t = [sb(f"mrow{i}", [1, 512], F32) for i in range(2)]
    ada_cnt = {"n": 0}

    def adaln_tile(j12):
        i = ada_cnt["n"]; ada_cnt["n"] += 1
        t = wada_t[i % 2]; key = ("wada%d" % (i % 2),)
        dma("pool", t, wada_v[:, :, j12 * 512:(j12 + 1) * 512], writes=[key])
        is_gate = j12 in (4, 5, 10, 11)
        if not is_gate:
            for jj in range(4):
                j = j12 * 4 + jj
                for k in range(8):
                    P.add("pe", lambda e, k=k, j=j, jj=jj: e.matmul(bank(7)[:, j:j + 1], lhsT=t[:, k, jj * 128:(jj + 1) * 128],
                                                                    rhs=c_bf[:, k:k + 1], start=(k == 0), stop=(k == 7)),
                          reads=[key, "c_bf"], writes=[("ps", 7)])
            j0 = j12 * 4
            P.add("dve", lambda e: e.tensor_tensor(out=modT[:, j0:j0 + 4], in0=bank(7)[:, j0:j0 + 4], in1=bT[:, j0:j0 + 4], op=ALU.add),
                  reads=[("ps", 7), "bT"], writes=[("modT", j0 + jj) for jj in range(4)])
        else:
            br = brow_t[i % 2]; mr = mrow_t[i % 2]
            gi = {4: 0, 5: 1, 10: 2, 11: 3}[j12]
            dma("sp", br, b_ada[0:1, j12 * 512:(j12 + 1) * 512], writes=[("brow%d" % (i % 2),)])
            for k in range(8):
                P.add("pe", lambda e, k=k: e.matmul(bank(6)[0:1, :], lhsT=c_bf[:, k:k + 1], rhs=t[:, k, :],
                                                    start=(k == 0), stop=(k == 7)),
                      reads=[key, "c_bf"], writes=[("ps", 6)])
            P.add("dve", lambda e: e.tensor_tensor(out=mr, in0=bank(6)[0:1, :], in1=br, op=ALU.add),
                  reads=[("ps", 6), ("brow%d" % (i % 2),)], writes=[("mrow%d" % (i % 2),)])
            dma("sp", mod_scr[0:1, gi * 512:(gi + 1) * 512], mr, reads=[("mrow%d" % (i % 2),)], writes=[("mod_scr", gi)])

    for j12 in (0, 1, 2, 3):
        adaln_tile(j12)
    P.add("dve", lambda e: e.scalar_tensor_tensor(out=A1, in0=modT[:, 8:16], scalar=1.0, in1=g1T,
                                                  op0=ALU.add, op1=ALU.mult),
          reads=[("modT", j) for j in range(8, 16)] + ["g1T"], writes=["A1"])

    def B1(c):
        return modT[:, c:c + 1]

    def B2(c):
        return modT[:, 24 + c:25 + c]
    B1keys = [("modT", j) for j in range(8)]
    B2keys = [("modT", j) for j in range(24, 32)]

    xs_t = [sb(f"xs{i}", [128, D], F32) for i in range(3)]
    xn_t = [sb(f"xn{i}", [128, D], BF16) for i in range(3)]
    NXN = {"n": 3}
    junk = sb("junk", [128, D], BF16)
    stat = sb("stat", [128, 64], F32)
    cnt = {"xs": 0, "xn": 0, "st": 0, "tp": 0, "hT": 0}

    def rms_scale_block(xs, kx, nrows):
        i2 = cnt["xn"]; cnt["xn"] += 1
        xn = xn_t[i2 % 3]; kn = ("xn%d" % (i2 % 3),)
        s = cnt["st"] % 16; cnt["st"] += 1
        ks = ("stat", s)
        P.add("act", lambda e: e.activation(out=junk[0:nrows, :], in_=xs[0:nrows, :], func=AF.Square,
                                            accum_out=stat[0:nrows, 4 * s:4 * s + 1]),
              reads=[kx], writes=["junk", ks])
        P.add("act", lambda e: e.activation(out=stat[0:nrows, 4 * s + 1:4 * s + 2], in_=stat[0:nrows, 4 * s:4 * s + 1],
                                            func=AF.Ln, scale=1.0 / D, bias=eps_col[0:nrows, :]),
              reads=[ks, "eps"], writes=[ks])
        P.add("act", lambda e: e.activation(out=stat[0:nrows, 4 * s + 2:4 * s + 3], in_=stat[0:nrows, 4 * s + 1:4 * s + 2],
                                            func=AF.Exp, scale=-0.5),
              reads=[ks], writes=[ks])
        P.add("dve", lambda e: e.tensor_scalar(out=xn[0:nrows, :], in0=xs[0:nrows, :],
                                               scalar1=stat[0:nrows, 4 * s + 2:4 * s + 3], scalar2=None, op0=ALU.mult),
              reads=[kx, ks], writes=[kn])
        return xn, kn

    def transpose_A(xn, kn, nrows, tp_banks):
        tb = tp_banks[cnt["tp"] % len(tp_banks)]; cnt["tp"] += 1
        ktp = ("ps", tb)
        tpb = bank_bf(tb)
        for c in range(8):
            P.add("pe", lambda e, c=c: e.transpose(out=tpb[:, c * 128:c * 128 + nrows],
                                                   in_=xn[0:nrows, c * 128:(c + 1) * 128],
                                                   identity=ident[0:nrows, 0:nrows]),
                  reads=[kn, "ident"], writes=[ktp])
        return tpb, ktp

    def transpose_B(tpb, ktp, nrows, hT, hkey, col0, Acols, Akey, Bfn, Bkeys, act_share=4):
        for c in range(8):
            dst = hT[:, c, col0:col0 + nrows]
            src = tpb[:, c * 128:c * 128 + nrows]
            if c < act_share:
                P.add("act", lambda e, c=c, dst=dst, src=src: e.activation(out=dst, in_=src, func=AF.Identity,
                                                                            scale=Acols[:, c:c + 1], bias=Bfn(c)),
                      reads=[ktp, Akey] + Bkeys, writes=[hkey])
            else:
                P.add("dve", lambda e, c=c, dst=dst, src=src: e.tensor_scalar(out=dst, in0=src, scalar1=Acols[:, c:c + 1],
                                                                               scalar2=Bfn(c), op0=ALU.mult, op1=ALU.add),
                      reads=[ktp, Akey] + Bkeys, writes=[hkey])

    def transpose_mod(xn, kn, nrows, hT, hkey, col0, Acols, Akey, Bfn, Bkeys, tp_banks):
        tpb, ktp = transpose_A(xn, kn, nrows, tp_banks)
        transpose_B(tpb, ktp, nrows, hT, hkey, col0, Acols, Akey, Bfn, Bkeys, act_share=6)

    def norm_A1(src_ap, nrows):
        i = cnt["xs"]; cnt["xs"] += 1
        xs = xs_t[i % 3]; kx = ("xs%d" % (i % 3),)
        dma("sp", xs[0:nrows, :], src_ap, writes=[kx])
        return rms_scale_block(xs, kx, nrows)

    def norm_A2(ctx, nrows, tp_banks=(0, 1)):
        return transpose_A(ctx[0], ctx[1], nrows, tp_banks)

    def norm_B(ctx, nrows, hT, hkey, col0):
        transpose_B(ctx[0], ctx[1], nrows, hT, hkey, col0, A1, "A1", B1, B1keys, act_share=3)

    def run_blocks(items, skew1=2, skew2=0):
        blks = [it for it in items if it[0] == "blk"]
        c1 = {}
        c2 = {}
        st_ = {"n1": 0, "n2": 0}

        def ensure1(upto):
            while st_["n1"] <= upto and st_["n1"] < len(blks):
                b = blks[st_["n1"]]
                c1[st_["n1"]] = norm_A1(b[1], b[2])
                st_["n1"] += 1

        def ensure2(upto):
            while st_["n2"] <= upto and st_["n2"] < len(blks):
                n = st_["n2"]
                ensure1(n)
                c2[n] = norm_A2(c1.pop(n), blks[n][2])
                st_["n2"] += 1
        bi = 0
        for it in items:
            if it[0] == "blk":
                ensure1(bi + skew1)
                ensure2(bi + skew2)
                norm_B(c2.pop(bi), it[2], it[3], it[4], it[5])
                bi += 1
            else:
                it[1]()

    win_v = w_in.rearrange("(k p) n -> p k n", p=128)
    wqkvu = sb("wqkvu", [128, 8, 2048], BF16)
    for j in (1, 2, 0, 3):
        dma("pool", wqkvu[:, :, j * 512:(j + 1) * 512], win_v[:, :, j * 512:(j + 1) * 512], writes=[("wqkvu", j)])
    gq_col = sb("gq_raw", [128, 1], F32)
    gk_col = sb("gk", [128, 1], F32)
    gqs_col = sb("gq", [128, 1], F32)
    dma("sp", gq_col, g_q, writes=["gq_raw"])
    dma("sp", gk_col, g_k, writes=["gk"])
    P.add("dve", lambda e: e.tensor_scalar(out=gqs_col, in0=gq_col, scalar1=0.125, scalar2=None, op0=ALU.mult),
          reads=["gq_raw"], writes=["gq"])
    gk8_col = sb("gk8", [128, 1], F32)
    P.add("dve", lambda e: e.tensor_scalar(out=gk8_col, in0=gk_col, scalar1=8.0, scalar2=None, op0=ALU.mult),
          reads=["gk"], writes=["gk8"])

    KT = sb("KT", [128, 4, S], BF16)
    V = sb("V", [128, 32, 512], BF16)
    hT_t = [sb(f"hT{i}", [128, 8, 512], BF16) for i in range(2)]
    sq_t = [sb(f"sq{i}", [128, 512], BF16) for i in range(2)]
    ln_t = [sb(f"lt{i}", [128, 512], F32) for i in range(2)]
    rs_t = [sb(f"rs{i}", [128, 512], F32) for i in range(2)]
    cq = {"n": 0}

    def headnorm_p1(hT, hkey, wcol0, wkey, hp):
        i = cq["n"]; cq["n"] += 1
        pb = (2, 3, 5)[i % 3]
        kpb = ("ps", pb)
        sq = sq_t[i % 2]
        ksq = ("sq%d" % (i % 2),)
        for k in range(8):
            P.add("pe", lambda e, k=k: e.matmul(bank(pb), lhsT=wqkvu[:, k, wcol0 + hp * 128:wcol0 + (hp + 1) * 128],
                                                rhs=hT[:, k, :], start=(k == 0), stop=(k == 7)),
                  reads=[wkey, hkey], writes=[kpb])
        P.add("act", lambda e: e.activation(out=sq, in_=bank(pb), func=AF.Square), reads=[kpb], writes=[ksq])
        return (i, hp)

    def headnorm_p2(ctx, gcol, gkey, dst, dkey_fn, col0):
        i, hp = ctx
        pb = (2, 3, 5)[i % 3]
        kpb = ("ps", pb)
        kss = ("ps", 4)
        ssb = bank(4)
        sq = sq_t[i % 2]; lt = ln_t[i % 2]; rs = rs_t[i % 2]
        ksq = ("sq%d" % (i % 2),); klt = ("lt%d" % (i % 2),); krs = ("rs%d" % (i % 2),)
        P.add("pe", lambda e: e.matmul(ssb, lhsT=blk, rhs=sq, start=True, stop=True), reads=[ksq, "blk"], writes=[kss])
        P.add("act", lambda e: e.activation(out=lt, in_=ssb, func=AF.Ln, scale=1.0 / 64, bias=eps_col),
              reads=[kss, "eps"], writes=[klt])
        P.add("act", lambda e: e.activation(out=rs, in_=lt, func=AF.Exp, scale=-0.5), reads=[klt], writes=[krs])
        P.add("dve", lambda e: e.scalar_tensor_tensor(out=dst[:, hp, col0:col0 + 512], in0=bank(pb),
                                                      scalar=gcol[:, 0:1], in1=rs, op0=ALU.mult, op1=ALU.mult),
              reads=[kpb, krs, gkey], writes=[dkey_fn(hp)])

    items = []
    n_all_tiles = 8 if stage >= 1 else 0

    hn_ctx = {}

    def piece1a(T, i, hT, hkey):
        hn_ctx[("k", T, i)] = headnorm_p1(hT, hkey, 512, ("wqkvu", 1), i)
        vb = 6
        for k in range(8):
            P.add("pe", lambda e, k=k: e.matmul(bank(vb), lhsT=hT[:, k, i * 128:(i + 1) * 128],
                                                rhs=wqkvu[:, k, 1024:1536], start=(k == 0), stop=(k == 7)),
                  reads=[("wqkvu", 2), hkey], writes=[("ps", vb)])
        P.add("dve", lambda e: e.tensor_copy(out=V[:, T * 4 + i, :], in_=bank(vb)),
              reads=[("ps", vb)], writes=[("V", T * 4 + i)])
        if i == 3:
            adaln_tile(4 + T)

    def piece1b(T, i):
        headnorm_p2(hn_ctx.pop(("k", T, i)), gk_col, "gk", KT, lambda hp: ("KT", hp, T), T * 512)

    pieces1 = []
    blk_items1 = []
    for T in range(n_all_tiles):
        hT = hT_t[cnt["hT"] % 2]; hkey = ("hT%d" % (cnt["hT"] % 2),); cnt["hT"] += 1
        for b4 in range(4):
            r0 = T * 512 + b4 * 128
            blk_items1.append(("blk", x_all[r0:r0 + 128, :], 128, hT, hkey, b4 * 128))
            pieces1.append((T, b4, hT, hkey))
    for n in range(len(blk_items1) + 5):
        if n < len(blk_items1):
            items.append(blk_items1[n])
        if 0 <= n - 4 < len(pieces1):
            items.append(("fn", lambda a=pieces1[n - 4]: piece1a(*a)))
        if 0 <= n - 5 < len(pieces1):
            items.append(("fn", lambda a=pieces1[n - 5]: piece1b(a[0], a[1])))

    def post_ada():
        P.add("dve", lambda e: e.scalar_tensor_tensor(out=A2, in0=modT[:, 32:40], scalar=1.0, in1=g2T,
                                                      op0=ALU.add, op1=ALU.mult),
              reads=[("modT", j) for j in range(32, 40)] + ["g2T"], writes=["A2"])
        dma("sp", gate1_bc, mod_scr[0:1, 0:D].to_broadcast([128, D]), reads=[("mod_scr", 0), ("mod_scr", 1)], writes=["gate1_bc"])
        dma("sp", gate2_bc, mod_scr[0:1, D:2 * D].to_broadcast([128, D]), reads=[("mod_scr", 2), ("mod_scr", 3)], writes=["gate2_bc"])

    if stage >= 1:
        items.append(("fn", post_ada))
    if stage < 2:
        run_blocks(items)

    if stage >= 2:
        AR.release("wada0", "wada1", "brow0", "brow1", "mrow0", "mrow1")
        QT = sb("QT", [128, 4, TOK_OWN], BF16)
        pooledT = sb("pooledT", [128, 4, TOK_OWN], BF16)
        wpg = sb("wpg", [128, 4, 128], BF16)
        pscale = sb("pscale", [128, 4], F32)
        hmask = sb("hmask", [128, 64], F32)
        icnt = sb("icnt", [128, 64], F32)
        uhalo = sb("uhalo", [128, 4, 64], F32)
        hTh = sb("hTh", [128, 8, 64], BF16)
        uext = [sb(f"uext{i}", [128, 528], F32) for i in range(2)]
        sAB = [sb("sA", [128, 528], F32), sb("sB", [128, 528], F32)]
        mixed = [sb(f"mixed{i}", [128, 512], BF16) for i in range(2)]
        tmp16 = sb("tmp16", [128, 16], F32)
        def setup2():
            dma("pool", wpg, w_pool_group.rearrange("g c d -> c g d"), writes=["wpg"])
            dma("sp", pscale, pool_scale, writes=["pscale"])
            dma("sp", hmask, halo_mask, writes=["hmask"])
            dma("sp", icnt, invcnt, writes=["icnt"])
            P.add("pool", lambda e: e.memset(sAB[0], 0.0), writes=["sA"])
            P.add("pool", lambda e: e.memset(sAB[1], 0.0), writes=["sB"])

        def halo_proj():
            for g in range(4):
                ub = 2 + g % 2
                for k in range(8):
                    P.add("pe", lambda e, k=k, g=g, ub=ub: e.matmul(bank(ub)[:, 0:64], lhsT=wqkvu[:, k, 1536 + g * 128:1536 + (g + 1) * 128],
                                                                    rhs=hTh[:, k, 0:64], start=(k == 0), stop=(k == 7)),
                          reads=[("wqkvu", 3), ("hTh",)], writes=[("ps", ub)])
                P.add("dve", lambda e, g=g, ub=ub: e.tensor_tensor(out=uhalo[:, g, :], in0=bank(ub)[:, 0:64], in1=hmask, op=ALU.mult),
                      reads=[("ps", ub), "hmask"], writes=[("uhalo", g)])

        pend_wpg = []

        def flush_wpg():
            while pend_wpg:
                g, kt, mx, kmx = pend_wpg.pop(0)
                gb_ = 7
                P.add("pe", lambda e, g=g, mx=mx, gb_=gb_: e.matmul(bank(gb_), lhsT=wpg[:, g, :], rhs=mx, start=True, stop=True),
                      reads=["wpg", kmx], writes=[("ps", gb_)])
                P.add("act", lambda e, g=g, gb_=gb_, kt=kt: e.activation(out=pooledT[:, g, kt * 512:(kt + 1) * 512], in_=bank(gb_),
                                                                         func=AF.Copy, scale=pscale[:, g:g + 1]),
                      reads=[("ps", gb_), "pscale"], writes=[("pooledT", g, kt)])

        def piece2b(kt, g):
            headnorm_p2(hn_ctx.pop(("q", kt, g)), gqs_col, "gq", QT, lambda hp: ("QT", hp, kt), kt * 512)

        def piece2a(kt, g, hT, hkey):
            if g == 0:
                dma("sp", hT_scr[kt], hT.rearrange("p a b -> p (a b)"), reads=[hkey], writes=[("hT_scr", kt)])
            hn_ctx[("q", kt, g)] = headnorm_p1(hT, hkey, 0, ("wqkvu", 0), g)
            ue = uext[g % 2]; kue = ("uext%d" % (g % 2),)
            ub = 6
            for k in range(8):
                P.add("pe", lambda e, k=k: e.matmul(bank(ub), lhsT=wqkvu[:, k, 1536 + g * 128:1536 + (g + 1) * 128],
                                                    rhs=hT[:, k, :], start=(k == 0), stop=(k == 7)),
                      reads=[("wqkvu", 3), hkey], writes=[("ps", ub)])
            flush_wpg()
            P.add("act", lambda e: e.activation(out=ue[:, 16:528], in_=bank(ub), func=AF.Copy),
                  reads=[("ps", ub)], writes=[kue])
            P.add("pool", lambda e: e.tensor_copy(out=ue[:, 0:16], in_=uhalo[:, g, kt * 16:(kt + 1) * 16]),
                  reads=[("uhalo", g)], writes=[kue])
            cur = ue; kcur = kue
            for j in range(g + 1):
                dst = sAB[j % 2]; kd = "sA" if j % 2 == 0 else "sB"
                sh = 1 << j
                P.add("pool", lambda e, dst=dst, cur=cur, sh=sh: e.tensor_tensor(out=dst[:, sh:528], in0=cur[:, sh:528],
                                                                                in1=cur[:, 0:528 - sh], op=ALU.add),
                      reads=[kcur], writes=[kd])
                cur = dst; kcur = kd
            W = float(1 << (g + 1))
            mx = mixed[g % 2]; kmx = ("mixed%d" % (g % 2),)
            P.add("dve", lambda e, cur=cur: e.scalar_tensor_tensor(out=mx, in0=cur[:, 16:528], scalar=1.0 / W,
                                                                   in1=ue[:, 16:528], op0=ALU.mult, op1=ALU.subtract),
                  reads=[kcur, kue], writes=[kmx])
            if kt == 0:
                P.add("dve", lambda e, cur=cur: e.tensor_tensor(out=tmp16, in0=cur[:, 16:32], in1=icnt[:, g * 16:(g + 1) * 16], op=ALU.mult),
                      reads=[kcur, "icnt"], writes=["tmp16"])
                P.add("dve", lambda e: e.tensor_tensor(out=mx[:, 0:16], in0=tmp16, in1=ue[:, 16:32], op=ALU.subtract),
                      reads=["tmp16", kue], writes=[kmx])
            pend_wpg.append((g, kt, mx, kmx))

        items.append(("fn", setup2))
        items.append(("blk", x_halo[0:64, :], 64, hTh, ("hTh",), 0))
        items.append(("fn", halo_proj))
        pieces2 = []
        blk_items2 = []
        for kt in range(NT_OWN):
            hT = hT_t[cnt["hT"] % 2]; hkey = ("hT%d" % (cnt["hT"] % 2),); cnt["hT"] += 1
            for b4 in range(4):
                r0 = kt * 512 + b4 * 128
                blk_items2.append(("blk", x_own[r0:r0 + 128, :], 128, hT, hkey, b4 * 128))
                pieces2.append((kt, b4, hT, hkey))
        for n in range(len(blk_items2) + 5):
            if n < len(blk_items2):
                items.append(blk_items2[n])
            if 0 <= n - 4 < len(pieces2):
                items.append(("fn", lambda a=pieces2[n - 4]: piece2a(*a)))
            if 0 <= n - 5 < len(pieces2):
                items.append(("fn", lambda a=pieces2[n - 5]: piece2b(a[0], a[1])))
        items.append(("fn", flush_wpg))
        run_blocks(items)

    if stage >= 3:
        AR.release("wqkvu", "xs0", "xs1", "xs2", "xn0", "xn1", "xn2", "junk", "hT0", "hT1", "sq0", "sq1", "lt0", "lt1", "rs0", "rs1",
                   "uext0", "uext1", "sA", "sB", "mixed0", "mixed1", "tmp16", "hTh", "uhalo", "wpg", "hmask", "icnt")
        wsb = sb("wsb", [128, 4, D], BF16)
        wpo = sb("wpo", [128, 4, D], BF16)
        wout = sb("wout", [128, 8, D], BF16)
        wr = sb("wr", [128, 8, 64], BF16)
        rbias = sb("rbias", [128, 64], F32)
        mkb = sb("mkb", [128, 8, 512], BF16)
        dma("pool", mkb, maskb.rearrange("p (a b) -> p a b", b=512), writes=["mkb"])
        dma("pool", wout, w_out.rearrange("(c p) d -> p c d", p=128), writes=["wout"])
        dma("pool", wsb, w_sb_out.rearrange("(h p) d -> p h d", p=128), writes=["wsb"])
        dma("pool", wpo, w_pool_out.rearrange("(h p) d -> p h d", p=128), writes=["wpo"])
        dma("pool", wr, w_router.rearrange("(c p) e -> p c e", p=128), writes=["wr"])
        dma("sp", rbias, router_bias[0:1, :].to_broadcast([128, 64]), writes=["rbias"])
        for c in range(8):
            P.add("pool", lambda e, c=c: e.tensor_tensor(out=wout[:, c, :], in0=wout[:, c, :], in1=gate1_bc, op=ALU.mult),
                  reads=["wout", "gate1_bc"], writes=["wout"])
        oT = sb("oT", [128, 4, TOK_OWN], BF16)
        eG = [sb(f"eG{g}", [128, 1024], F32) for g in range(2)]
        spG = [sb(f"spG{g}", [128, 1024], BF16) for g in range(2)]
        aG = [sb(f"aG{g}", [128, 1024], BF16) for g in range(2)]
        SbG = [[sb(f"SbG{g}{s_}", [128, 1024], BF16) for s_ in range(2)] for g in range(2)]

        def PGb(j, g):
            if g == 0:
                return (0, 1) if j % 2 == 0 else (6, 7)
            return (2, 3)

        def PGv(j, g):
            b0 = PGb(j, g)[0]
            return psum[:, b0 * 512:(b0 + 2) * 512]
        grp_i = 0
        n_kt = NT_OWN if stage >= 4 or not dbg else 2
        for kt in range(n_kt):
            nst = 8 * (kt + 1)
            for hq in range(2):
                obase = 4
                grp_i += 1

                def DUM(n):
                    for q in range(n):
                        P.add("pe", lambda e, q=q: e.matmul(bank(6 + q % 2), lhsT=ident, rhs=mkb[:, q % 8, :], start=True, stop=True),
                              reads=["ident", "mkb"], writes=[("ps", 6 + q % 2)])

                def Zg(j, g, kt=kt, hq=hq):
                    kb = 8 * kt + 7 - j
                    hp = 2 * hq + g
                    masked = j < 8
                    for c in range(2):
                        pb = PGb(j, g)[c]
                        P.add("pe", lambda e, c=c, pb=pb: e.matmul(bank(pb), lhsT=KT[64 * c:64 * c + 64, hp, kb * 128:(kb + 1) * 128],
                                                                   rhs=QT[64 * c:64 * c + 64, hp, kt * 512:(kt + 1) * 512],
                                                                   start=True, stop=(not masked)),
                              reads=[("KT", hp, kb // 4), ("QT", hp, kt)], writes=[("ps", pb)])
                        if masked:
                            P.add("pe", lambda e, pb=pb: e.matmul(bank(pb), lhsT=ident, rhs=mkb[:, j, :], start=False, stop=True),
                                  reads=["ident", "mkb"], writes=[("ps", pb)])

                def Eg(j, g):
                    P.add("act", lambda e: e.activation(out=eG[g], in_=PGv(j, g), func=AF.Exp),
                          reads=[("ps", PGb(j, g)[0]), ("ps", PGb(j, g)[1])], writes=[(f"eG{g}",)])

                def Lg(j, g):
                    P.add("act", lambda e: e.activation(out=spG[g], in_=eG[g], func=AF.Ln, bias=one_col),
                          reads=[(f"eG{g}",), "one"], writes=[(f"spG{g}",)])

                def NOg(j, g):
                    if j == 0:
                        return
                    sbf = SbG[g][(j - 1) % 2]
                    for c in range(2):
                        pb = PGb(j, g)[c]
                        P.add("pe", lambda e, c=c, pb=pb, sbf=sbf: e.matmul(bank(pb), lhsT=nones, rhs=sbf[:, c * 512:(c + 1) * 512],
                                                                            start=False, stop=False, skip_group_check=True),
                              reads=["nones", (f"SbG{g}{(j - 1) % 2}",)], writes=[("ps", pb)])

                def TRg(j, g):
                    for c in range(2):
                        pb = PGb(j, g)[c]
                        P.add("pe", lambda e, c=c, pb=pb: e.matmul(bank(pb), lhsT=ntri, rhs=spG[g][:, c * 512:(c + 1) * 512],
                                                                   start=False, stop=True, skip_group_check=True),
                              reads=["ntri", (f"spG{g}",)], writes=[("ps", pb)])

                def SUg(j, g, nst=nst):
                    if j >= nst - 1:
                        return
                    if j == 0:
                        P.add("dve", lambda e: e.tensor_copy(out=SbG[g][0], in_=spG[g]), reads=[(f"spG{g}",)], writes=[(f"SbG{g}0",)])
                    else:
                        so = SbG[g][(j - 1) % 2]
                        P.add("dve", lambda e: e.tensor_tensor(out=SbG[g][j % 2], in0=so, in1=spG[g], op=ALU.add),
                              reads=[(f"SbG{g}{(j - 1) % 2}",), (f"spG{g}",)], writes=[(f"SbG{g}{j % 2}",)])

                def Xg(j, g):
                    P.add("act", lambda e: e.activation(out=aG[g], in_=PGv(j, g), func=AF.Exp),
                          reads=[("ps", PGb(j, g)[0]), ("ps", PGb(j, g)[1])], writes=[(f"aG{g}",)])

                def AVg(j, g, kt=kt, hq=hq, nst=nst, obase=obase):
                    kb = 8 * kt + 7 - j
                    hp = 2 * hq + g
                    ob = obase + g
                    for c in range(2):
                        h = 2 * hp + c
                        tp = (0, 64) if c == 1 else None
                        P.add("pe", lambda e, c=c, h=h, tp=tp: e.matmul(bank(ob)[64 * c:64 * c + 64, :], lhsT=V[:, kb, h * 64:(h + 1) * 64],
                                                                        rhs=aG[g][:, c * 512:(c + 1) * 512],
                                                                        start=(j == 0), stop=(j == nst - 1), tile_position=tp),
                              reads=[("V", kb), (f"aG{g}",)], writes=[("ps", ob, c)])

                Zg(0, 0); Zg(0, 1)
                Eg(0, 0); Eg(0, 1); Lg(0, 0); Lg(0, 1)
                for j in range(nst):
                    TRg(j, 0); SUg(j, 0)
                    TRg(j, 1); SUg(j, 1)
                    if j + 1 < nst:
                        Zg(j + 1, 0)
                    Xg(j, 0)
                    AVg(j, 0)
                    Xg(j, 1)
                    if j + 1 < nst:
                        Zg(j + 1, 1)
                    AVg(j, 1)
                    if j + 1 < nst:
                        Eg(j + 1, 0); NOg(j + 1, 0)
                        Eg(j + 1, 1); NOg(j + 1, 1)
                        Lg(j + 1, 0); Lg(j + 1, 1)
                for g in range(2):
                    hp = 2 * hq + g
                    ob = obase + g
                    P.add("dve", lambda e, ob=ob, hp=hp, kt=kt: e.tensor_copy(out=oT[:, hp, kt * 512:(kt + 1) * 512], in_=bank(ob)),
                          reads=[("ps", ob, 0), ("ps", ob, 1)], writes=[("oT", hp, kt)])

    BIG = 1.0e4
    if stage >= 4:
        AR.release("KT", "V", "QT", "mkb")
        AR.release("eG0", "eG1", "spG0", "spG1", "aG0", "aG1", "SbG00", "SbG01", "SbG10", "SbG11")
        h2T = sb("h2T", [128, 8, TOK_OWN], BF16)
        gatesT = sb("gatesT", [64, TOK_OWN], BF16)
        wgab_t = [sb(f"wgab{i}", [128, 8, 256], BF16) for i in range(4)]
        hTm_t = [sb(f"hTm{i}", [128, 8, 512], BF16) for i in range(2)]
        siga_t = [sb(f"siga{i}", [128, 512], F32) for i in range(2)]
        sigb_t = [sb(f"sigb{i}", [128, 512], F32) for i in range(2)]
        m1_t = [sb(f"m1{i}", [128, 512], F32) for i in range(2)]
        m2_t = [sb(f"m2{i}", [128, 512], F32) for i in range(2)]
        merged = sb("merged", [128, 8, 512], BF16)
        xo_t = [sb(f"xo{i}", [128, D], F32) for i in range(3)]
        xn_t[0] = sb("xn0b", [128, D], BF16); xn_t[1] = sb("xn1b", [128, D], BF16)
        junk4 = sb("junkb", [128, D], BF16)
        rt = {nm: sb("rt_" + nm, [128, 64], F32) for nm in ("sc", "sel", "g8", "selm", "emask", "w")}
        rs8 = {nm: sb("rs_" + nm, [128, 8], F32) for nm in ("grp", "gs", "gmask", "pen", "top8")}
        rs1 = {nm: sb("r1_" + nm, [128, 1], F32) for nm in ("wsum", "rws")}
        gates_bf = sb("gates_bf", [128, 64], BF16)

        def rms2(xs, kx, nrows):
            i2 = cnt["xn"]; cnt["xn"] += 1
            xn = xn_t[i2 % 2]; kn = ("xn%db" % (i2 % 2),)
            s_ = cnt["st"] % 16; cnt["st"] += 1
            ks = ("stat", s_)
            P.add("act", lambda e: e.activation(out=junk4, in_=xs, func=AF.Square, accum_out=stat[:, 4 * s_:4 * s_ + 1]),
                  reads=[kx], writes=["junkb", ks])
            P.add("pool", lambda e: e.tensor_tensor(out=stat[:, 4 * s_ + 1:4 * s_ + 2], in0=stat[:, 4 * s_:4 * s_ + 1],
                                                    in1=deps_col, op=ALU.add),
                  reads=[ks, "deps"], writes=[ks])
            P.add("pool", lambda e: e.tensor_tensor(out=stat[:, 4 * s_ + 2:4 * s_ + 3], in0=stat[:, 4 * s_ + 1:4 * s_ + 2],
                                                    in1=mhalf_col, op=ALU.pow),
                  reads=[ks, "mhalf"], writes=[ks])
            P.add("dve", lambda e: e.tensor_scalar(out=xn, in0=xs, scalar1=stat[:, 4 * s_ + 2:4 * s_ + 3], scalar2=32.0,
                                                   op0=ALU.mult, op1=ALU.mult),
                  reads=[kx, ks], writes=[kn])
            return xn, kn

        merged_t = [merged, sb("merged1", [128, 8, 512], BF16)]
        cnt4 = {"ci": 0, "xi": 0}

        def load_hTm(kt):
            hTm = hTm_t[kt % 2]; khm = ("hTm%d" % (kt % 2),)
            dma("sp", hTm.rearrange("p a b -> p (a b)"), hT_scr[kt], reads=[("hT_scr", kt)], writes=[khm])

        chunk_dma_done = {}

        def chunk4_dma(kt, c, ci):
            wg_ = wgab_t[ci % 4]; kwg = ("wgab%d" % (ci % 4),)
            dma("pool", wg_[:, :, 0:128], win_v[:, :, 2048 + c * 128:2048 + (c + 1) * 128], writes=[kwg])
            dma("pool", wg_[:, :, 128:256], win_v[:, :, 3072 + c * 128:3072 + (c + 1) * 128], writes=[kwg])
            chunk_dma_done[(kt, c)] = ci

        def chunk4(kt, c):
            hTm = hTm_t[kt % 2]; khm = ("hTm%d" % (kt % 2),)
            mg = merged_t[kt % 2]; kmg = "merged" if kt % 2 == 0 else "merged1"
            ci = cnt4["ci"]; cnt4["ci"] += 1
            wg_ = wgab_t[ci % 4]; kwg = ("wgab%d" % (ci % 4),)
            sa_ = siga_t[ci % 2]; ksa = ("siga%d" % (ci % 2),)
            sb_ = sigb_t[ci % 2]; ksb = ("sigb%d" % (ci % 2),)
            m1 = m1_t[ci % 2]; km1 = ("m1%d" % (ci % 2),)
            m2 = m2_t[ci % 2]; km2 = ("m2%d" % (ci % 2),)
            if (kt, c) not in chunk_dma_done:
                chunk4_dma(kt, c, ci)
            for k in range(8):
                P.add("pe", lambda e, k=k: e.matmul(bank(0), lhsT=wg_[:, k, 0:128], rhs=hTm[:, k, :], start=(k == 0), stop=(k == 7)),
                      reads=[kwg, khm], writes=[("ps", 0)])
            for k in range(8):
                P.add("pe", lambda e, k=k: e.matmul(bank(1), lhsT=wg_[:, k, 128:256], rhs=hTm[:, k, :], start=(k == 0), stop=(k == 7)),
                      reads=[kwg, khm], writes=[("ps", 1)])
            P.add("act", lambda e: e.activation(out=sa_, in_=bank(0), func=AF.Sigmoid), reads=[("ps", 0)], writes=[ksa])
            P.add("act", lambda e: e.activation(out=sb_, in_=bank(1), func=AF.Sigmoid), reads=[("ps", 1)], writes=[ksb])
            for hp in range(4):
                P.add("pe", lambda e, hp=hp: e.matmul(bank(2), lhsT=wsb[:, hp, c * 128:(c + 1) * 128],
                                                      rhs=oT[:, hp, kt * 512:(kt + 1) * 512], start=(hp == 0), stop=(hp == 3)),
                      reads=["wsb", ("oT", hp, kt)], writes=[("ps", 2)])
            for g in range(4):
                P.add("pe", lambda e, g=g: e.matmul(bank(3), lhsT=wpo[:, g, c * 128:(c + 1) * 128],
                                                    rhs=pooledT[:, g, kt * 512:(kt + 1) * 512], start=(g == 0), stop=(g == 3)),
                      reads=["wpo", ("pooledT", g, kt)], writes=[("ps", 3)])
            P.add("dve", lambda e: e.tensor_tensor(out=m1, in0=bank(2), in1=sa_, op=ALU.mult), reads=[("ps", 2), ksa], writes=[km1])
            P.add("dve", lambda e: e.tensor_tensor(out=m2, in0=bank(3), in1=sb_, op=ALU.mult), reads=[("ps", 3), ksb], writes=[km2])
            P.add("dve", lambda e: e.tensor_tensor(out=mg[:, c, :], in0=m1, in1=m2, op=ALU.add), reads=[km1, km2], writes=[(kmg, c)])

        def block4_A(kt, b4):
            mg = merged_t[kt % 2]; kmg = "merged" if kt % 2 == 0 else "merged1"
            blk_i = kt * 4 + b4
            xi = cnt4["xi"]; cnt4["xi"] += 1
            xo = xo_t[xi % 3]; kxo = ("xo%d" % (xi % 3),)
            r0 = blk_i * 128
            dma("sp", xo, x_own[r0:r0 + 128, :], writes=[kxo])
            for hf in range(2):
                for c in range(8):
                    P.add("pe", lambda e, c=c, hf=hf: e.matmul(bank(4 + hf), lhsT=mg[:, c, b4 * 128:(b4 + 1) * 128],
                                                               rhs=wout[:, c, hf * 512:(hf + 1) * 512], start=(c == 0), stop=(c == 7)),
                          reads=[(kmg, c), "wout"], writes=[("ps", 4 + hf)])
            P.add("dve", lambda e: e.tensor_tensor(out=xo, in0=psum[:, 4 * 512:6 * 512], in1=xo, op=ALU.add),
                  reads=[("ps", 4), ("ps", 5), kxo], writes=[kxo])
            dma("sp", xnew_scr[r0:r0 + 128, :], xo, reads=[kxo], writes=[("xnew_scr", blk_i)])
            return rms2(xo, kxo, 128)

        def block4_B(kt, b4, xn, kn):
            blk_i = kt * 4 + b4
            transpose_mod(xn, kn, 128, h2T, ("h2T", blk_i), blk_i * 128, A2, "A2", B2, B2keys, (6,))

        def router4_A(kt, b4):
            blk_i = kt * 4 + b4
            lg = bank(7)[:, 0:64]
            for c in range(8):
                P.add("pe", lambda e, c=c: e.matmul(lg, lhsT=h2T[:, c, blk_i * 128:(blk_i + 1) * 128], rhs=wr[:, c, :],
                                                    start=(c == 0), stop=(c == 7)),
                      reads=[("h2T", blk_i), "wr"], writes=[("ps", 7)])
            sc, sel_, g8, selm, emask, w_ = (rt[n] for n in ("sc", "sel", "g8", "selm", "emask", "w"))
            grp, gs, gmask, pen, top8 = (rs8[n] for n in ("grp", "gs", "gmask", "pen", "top8"))
            wsum, rws = rs1["wsum"], rs1["rws"]
            P.add("act", lambda e: e.activation(out=sc, in_=lg, func=AF.Sigmoid), reads=[("ps", 7)], writes=["rt_sc"])
            P.add("dve", lambda e: e.tensor_tensor(out=sel_, in0=sc, in1=rbias, op=ALU.add), reads=["rt_sc", "rbias"], writes=["rt_sel"])
            for g in range(8):
                P.add("dve", lambda e, g=g: e.max(out=g8[:, g * 8:(g + 1) * 8], in_=sel_[:, g * 8:(g + 1) * 8]),
                      reads=["rt_sel"], writes=["rt_g8"])
            g8v = g8.rearrange("p (g e) -> p g e", e=8)
            P.add("dve", lambda e: e.tensor_tensor(out=grp, in0=g8v[:, :, 0], in1=g8v[:, :, 1], op=ALU.add),
                  reads=["rt_g8"], writes=["rs_grp"])
            P.add("dve", lambda e: e.max(out=gs, in_=grp), reads=["rs_grp"], writes=["rs_gs"])
            P.add("dve", lambda e: e.tensor_scalar(out=gmask, in0=grp, scalar1=gs[:, 3:4], scalar2=None, op0=ALU.is_ge),
                  reads=["rs_grp", "rs_gs"], writes=["rs_gmask"])
            P.add("dve", lambda e: e.tensor_scalar(out=pen, in0=gmask, scalar1=BIG, scalar2=-BIG, op0=ALU.mult, op1=ALU.add),
                  reads=["rs_gmask"], writes=["rs_pen"])
            P.add("dve", lambda e: e.tensor_tensor(out=selm.rearrange("p (g e) -> p g e", e=8), in0=sel_.rearrange("p (g e) -> p g e", e=8),
                                                   in1=pen.unsqueeze(2).to_broadcast([128, 8, 8]), op=ALU.add),
                  reads=["rt_sel", "rs_pen"], writes=["rt_selm"])
            P.add("dve", lambda e: e.max(out=top8, in_=selm), reads=["rt_selm"], writes=["rs_top8"])
            P.add("dve", lambda e: e.tensor_scalar(out=emask, in0=selm, scalar1=top8[:, 7:8], scalar2=None, op0=ALU.is_ge),
                  reads=["rt_selm", "rs_top8"], writes=["rt_emask"])
            P.add("dve", lambda e: e.tensor_tensor(out=w_, in0=sc, in1=emask, op=ALU.mult), reads=["rt_sc", "rt_emask"], writes=["rt_w"])
            P.add("dve", lambda e: e.reduce_sum(out=wsum, in_=w_, axis=mybir.AxisListType.X), reads=["rt_w"], writes=["r1_wsum"])
            P.add("dve", lambda e: e.reciprocal(out=rws, in_=wsum), reads=["r1_wsum"], writes=["r1_rws"])
            P.add("dve", lambda e: e.tensor_scalar(out=gates_bf, in0=w_, scalar1=rws[:, 0:1], scalar2=2.5, op0=ALU.mult, op1=ALU.mult),
                  reads=["rt_w", "r1_rws"], writes=["gates_bf"])

        def router4_B(kt, b4):
            blk_i = kt * 4 + b4
            gtp = bank_bf(7)[0:64, 256:384]
            P.add("pe", lambda e: e.transpose(out=gtp, in_=gates_bf, identity=ident), reads=["gates_bf", "ident"], writes=[("ps", 7)])
            P.add("act", lambda e: e.activation(out=gatesT[:, blk_i * 128:(blk_i + 1) * 128], in_=gtp, func=AF.Copy),
                  reads=[("ps", 7)], writes=[("gatesT", blk_i)])

        load_hTm(0)
        for c in range(8):
            chunk4(0, c)
        prev = None
        pend_rb = None
        defer_r = []
        for kt in range(NT_OWN):
            nxt = kt + 1 < NT_OWN
            if nxt:
                load_hTm(kt + 1)
            elif stage >= 5:
                AR.release("wgab0", "wgab1", "wgab2", "wgab3", "hTm0", "hTm1", "siga0", "siga1", "sigb0", "sigb1",
                           "m10", "m11", "m20", "m21")
                wg_e = sb("wg0", [128, 4, 8, 128], BF16)
                wu_e = sb("wu0", [128, 4, 8, 128], BF16)
                wd_e = sb("wd0", [128, 4, D], BF16)
                dma("pool", wg_e, w_exp_gate[0:4].rearrange("e (k p) f -> p e k f", p=128), writes=[("wg0",)])
                dma("pool", wu_e, w_exp_up[0:4].rearrange("e (k p) f -> p e k f", p=128), writes=[("wu0",)])
                dma("pool", wd_e, w_exp_down[0:4].rearrange("e f d -> f e d"), writes=[("wd0",)])
            for b4 in range(4):
                if nxt:
                    chunk4_dma(kt + 1, 2 * b4, cnt4["ci"])
                    chunk4_dma(kt + 1, 2 * b4 + 1, cnt4["ci"] + 1)
                xn_kn = block4_A(kt, b4)
                if prev is not None:
                    block4_B(*prev)
                if nxt:
                    chunk4(kt + 1, 2 * b4)
                if pend_rb is not None:
                    router4_B(*pend_rb)
                    pend_rb = None
                if prev is not None:
                    if stage >= 5 and (prev[0], prev[1]) == (NT_OWN - 1, 2):
                        defer_r.append((prev[0], prev[1]))
                    else:
                        router4_A(prev[0], prev[1])
                        pend_rb = (prev[0], prev[1])
                if nxt:
                    chunk4(kt + 1, 2 * b4 + 1)
                prev = (kt, b4) + xn_kn
        block4_B(*prev)
        if pend_rb is not None:
            router4_B(*pend_rb)
        if stage >= 5:
            defer_r.append((prev[0], prev[1]))
        else:
            router4_A(prev[0], prev[1])
            router4_B(prev[0], prev[1])

        def gates_store(t_):
            dma("sp", gates_scr[:, t_ * 512:(t_ + 1) * 512], gatesT[:, t_ * 512:(t_ + 1) * 512],
                reads=[("gatesT", t_ * 4 + q) for q in range(4)], writes=[("gates_scr", t_)])
        for t_ in range(3 if stage >= 5 else 4):
            gates_store(t_)

    if stage >= 5:
        AR.release("oT", "pooledT", "wsb", "wpo", "wout",
                   "merged", "merged1", "xo0", "xo1", "xo2", "xn0b", "xn1b", "junkb")
        acc_t = [sb(f"acc{q}", [128, 4, D], F32) for q in range(4)]
        gbc_t = [sb(f"gbc{q}", [128, 512], BF16) for q in range(4)]
        gbc_n = {"n": 0}

        def accv(b_):
            return acc_t[b_ // 4][:, b_ % 4, :]

        def acck(b_):
            return (f"acc{b_ // 4}", b_ % 4)
        wg_t = [wg_e, sb("wg1", [128, 4, 8, 128], BF16)]
        wu_t = [wu_e, sb("wu1", [128, 4, 8, 128], BF16)]
        wd_t = [wd_e, sb("wd1", [128, 4, D], BF16)]
        sa_t = [sb(f"sa{i}", [128, 512], F32) for i in range(2)]
        t1_t = [sb(f"t1{i}", [128, 512], F32) for i in range(2)]
        hm_t = [sb(f"hm{i}", [128, 4, 512], BF16) for i in range(2)]
        xf_t = [sb(f"xf{i}", [128, D], F32) for i in range(2)]
        n_grp = 17
        ei = 0
        ti = 0
        yi = {"n": 0}
        GBK = 0
        UBK = (1, 2)
        SBK = 3
        YBK = ((4, 5), (6, 7))

        def GU(grp_i, t, ne, wg_, wu_, kw, kwu, hm, khm_):
            nonlocal ei
            for el in range(ne):
                ab = ei % 2
                sa_ = sa_t[ab]; ksa = ("sa%d" % ab,)
                t1 = t1_t[ab]; kt1 = ("t1%d" % ab,)
                ub = UBK[ab]
                ei += 1
                for k in range(8):
                    P.add("pe", lambda e, k=k, el=el, wg_=wg_, t=t: e.matmul(bank(GBK), lhsT=wg_[:, el, k, :],
                                                                            rhs=h2T[:, k, t * 512:(t + 1) * 512], start=(k == 0), stop=(k == 7)),
                          reads=[kw] + [("h2T", t * 4 + q) for q in range(4)], writes=[("ps", GBK)])
                for k in range(8):
                    P.add("pe", lambda e, k=k, el=el, ub=ub, wu_=wu_, t=t: e.matmul(bank(ub), lhsT=wu_[:, el, k, :],
                                                                                   rhs=h2T[:, k, t * 512:(t + 1) * 512], start=(k == 0), stop=(k == 7)),
                          reads=[kwu] + [("h2T", t * 4 + q) for q in range(4)], writes=[("ps", ub)])
                P.add("act", lambda e, sa_=sa_: e.activation(out=sa_, in_=bank(GBK), func=AF.Silu), reads=[("ps", GBK)], writes=[ksa])
                if grp_i < 16:
                    eg = grp_i * 4 + el
                    gi_ = gbc_n["n"] % 4; gbc_n["n"] += 1
                    gb = gbc_t[gi_]; kgb = ("gbc%d" % gi_,)
                    dma("sp", gb, gates_scr[eg:eg + 1, t * 512:(t + 1) * 512].to_broadcast([128, 512]),
                        reads=[("gates_scr", t)], writes=[kgb])
                    P.add("dve", lambda e, t1=t1, sa_=sa_, ub=ub: e.tensor_tensor(out=t1, in0=bank(ub), in1=sa_, op=ALU.mult),
                          reads=[("ps", ub), ksa], writes=[kt1])
                    P.add("dve", lambda e, t1=t1, hm=hm, el=el, gb=gb: e.tensor_tensor(out=hm[:, el, :], in0=gb, in1=t1, op=ALU.mult),
                          reads=[kgb, kt1], writes=[(khm_, el)])
                else:
                    P.add("dve", lambda e, sa_=sa_, hm=hm, el=el, ub=ub: e.tensor_tensor(out=hm[:, el, :], in0=bank(ub), in1=sa_, op=ALU.mult),
                          reads=[("ps", ub), ksa], writes=[(khm_, el)])

        def DN(grp_i, t, ne, wd_, kwd, hm, khm_):
            for b4 in range(4):
                blk_i = t * 4 + b4
                yb = YBK[yi["n"] % 2]; yi["n"] += 1
                ybank = psum[:, yb[0] * 512:(yb[1] + 1) * 512]
                for el in range(ne):
                    for hf in range(2):
                        P.add("pe", lambda e, el=el, hf=hf, b4=b4, hm=hm, wd_=wd_, ne=ne, yb=yb: e.matmul(
                            bank(yb[hf]), lhsT=hm[:, el, b4 * 128:(b4 + 1) * 128], rhs=wd_[:, el, hf * 512:(hf + 1) * 512],
                            start=(el == 0), stop=(el == ne - 1)),
                              reads=[(khm_, el), kwd], writes=[("ps", yb[hf])])
                r0 = blk_i * 128
                if grp_i == 0:
                    xf = xf_t[blk_i % 2]; kxf = ("xf%d" % (blk_i % 2),)
                    dma("sp", xf, xnew_scr[r0:r0 + 128, :], reads=[("xnew_scr", blk_i)], writes=[kxf])
                    P.add("dve", lambda e, blk_i=blk_i, ybank=ybank, xf=xf: e.tensor_tensor(out=accv(blk_i), in0=ybank, in1=xf, op=ALU.add),
                          reads=[("ps", yb[0]), ("ps", yb[1]), kxf], writes=[acck(blk_i)])
                else:
                    P.add("dve", lambda e, blk_i=blk_i, ybank=ybank: e.tensor_tensor(out=accv(blk_i), in0=ybank, in1=accv(blk_i), op=ALU.add),
                          reads=[("ps", yb[0]), ("ps", yb[1]), acck(blk_i)], writes=[acck(blk_i)])
                if grp_i == n_grp - 1:
                    final_ops.append(dma("sp", out[r0:r0 + 128, :], accv(blk_i), reads=[acck(blk_i)]))

        pend = None
        for grp_i in range(n_grp):
            sl = grp_i % 2
            wg_, wu_, wd_ = wg_t[sl], wu_t[sl], wd_t[sl]
            kw = ("wg%d" % sl,); kwu = ("wu%d" % sl,); kwd = ("wd%d" % sl,)
            if grp_i == 0:
                ne = 4
            elif grp_i < 16:
                ne = 4
                e0 = grp_i * 4
                dma("pool", wg_, w_exp_gate[e0:e0 + 4].rearrange("e (k p) f -> p e k f", p=128), writes=[kw])
                dma("pool", wu_, w_exp_up[e0:e0 + 4].rearrange("e (k p) f -> p e k f", p=128), writes=[kwu])
                dma("pool", wd_, w_exp_down[e0:e0 + 4].rearrange("e f d -> f e d"), writes=[kwd])
            else:
                ne = 2
                dma("pool", wg_[:, 0:2], w_sh_gate.rearrange("(k p) (e f) -> p e k f", p=128, f=128), writes=[kw])
                dma("pool", wu_[:, 0:2], w_sh_up.rearrange("(k p) (e f) -> p e k f", p=128, f=128), writes=[kwu])
                dma("pool", wd_[:, 0:2], w_sh_down.rearrange("(e f) d -> f e d", f=128), writes=[kwd])
            for el in range(ne):
                P.add("pool", lambda e, el=el, wd_=wd_: e.tensor_tensor(out=wd_[:, el, :], in0=wd_[:, el, :], in1=gate2_bc, op=ALU.mult),
                      reads=[kwd, "gate2_bc"], writes=[kwd])
            for t in range(4):
                hm = hm_t[ti % 2]; khm_ = "hm%d" % (ti % 2)
                ti += 1
                GU(grp_i, t, ne, wg_, wu_, kw, kwu, hm, khm_)
                if pend is not None:
                    DN(*pend)
                pend = (grp_i, t, ne, wd_, kwd, hm, khm_)
                if grp_i == 0 and len(defer_r) == 2:
                    if t == 0:
                        router4_A(*defer_r[0])
                    elif t == 1:
                        router4_B(*defer_r[0])
                        router4_A(*defer_r[1])
                    elif t == 2:
                        router4_B(*defer_r[1])
                        gates_store(3)
        DN(*pend)

    if dbg and stage == 4:
        AR.release("oT", "pooledT", "wout")
        dtmp = sb("dtmp", [128, 4096], F32)
        P.add("dve", lambda e: e.tensor_copy(out=dtmp[:, 0:2048], in_=h2T[:, 0, :]),
              reads=[("h2T", b_) for b_ in range(16)], writes=["dtmp"])
        P.add("dve", lambda e: e.tensor_copy(out=dtmp[0:64, 2048:4096], in_=gatesT),
              reads=[("gatesT", b_) for b_ in range(16)], writes=["dtmp"])
        final_ops.append(dma("sp", dbg_out[:, 0:4096], dtmp, reads=["dtmp"]))
        final_ops.append(dma("sp", out, xnew_scr, reads=[("xnew_scr", b_) for b_ in range(16)]))

    if dbg and stage in (2, 3):
        if stage == 2:
            AR.release("wqkvu", "KT", "V")
        else:
            AR.release("KT", "V")
        dtmp = sb("dtmp", [128, 8192], F32)
        P.add("dve", lambda e: e.tensor_copy(out=dtmp[:, 0:2048], in_=QT[:, 0, :]),
              reads=[("QT", 0, kt) for kt in range(4)], writes=["dtmp"])
        P.add("dve", lambda e: e.tensor_copy(out=dtmp[:, 2048:4096], in_=pooledT[:, 0, :]),
              reads=[("pooledT", 0, kt) for kt in range(4)], writes=["dtmp"])
        P.add("dve", lambda e: e.tensor_copy(out=dtmp[:, 4096:6144], in_=pooledT[:, 3, :]),
              reads=[("pooledT", 3, kt) for kt in range(4)], writes=["dtmp"])
        if stage == 3:
            P.add("dve", lambda e: e.tensor_copy(out=dtmp[:, 6144:7168], in_=oT[:, 0, 0:1024]),
                  reads=[("oT", 0, kt) for kt in range(2)], writes=["dtmp"])
            P.add("dve", lambda e: e.tensor_copy(out=dtmp[:, 7168:8192], in_=oT[:, 3, 0:1024]),
                  reads=[("oT", 3, kt) for kt in range(2)], writes=["dtmp"])
        final_ops.append(dma("sp", dbg_out[:, 0:8192], dtmp, reads=["dtmp"]))

    if dbg and stage == 1:
        AR.release("wqkvu")
        dtmp = sb("dtmp", [128, 8192], F32)
        P.add("dve", lambda e: e.tensor_copy(out=dtmp[:, 0:4096], in_=KT[:, 0, :]),
              reads=[("KT", 0, T) for T in range(8)], writes=["dtmp"])
        P.add("dve", lambda e: e.tensor_copy(out=dtmp[:, 4096:8192], in_=V[:, 0:8, :].rearrange("p a b -> p (a b)")),
              reads=[("V", i) for i in range(8)], writes=["dtmp"])
        final_ops.append(dma("sp", dbg_out[:, 0:8192], dtmp, reads=["dtmp"]))
        dt2 = sb("dt2", [128, 64], F32)
        P.add("dve", lambda e: e.tensor_copy(out=dt2[:, 0:48], in_=modT), reads=[("modT", j) for j in range(16)], writes=["dt2"])
        final_ops.append(dma("sp", dbg_out[:, 8192:8192 + 48], dt2[:, 0:48], reads=["dt2"]))

    P.emit(nc, st, final_ops)
    P.stats["arena_peak"] = AR.peak
    st.close()
    return nc, P


def make_consts():
    ident = np.eye(128, dtype=np.float32)
    j = np.arange(128)[:, None]
    k = np.arange(128)[None, :]
    ntri = np.where(j >= k, -1.0, 0.0).astype(np.float32)
    blk = ((j // 64) == (k // 64)).astype(np.float32)
    sel = np.zeros((64, 64, 128), np.float32)
    sel[np.arange(64), np.arange(64), :] = 1.0
    return ident, ntri, blk, sel.reshape(64, 64 * 128)


def make_maskb(half):
    m = np.zeros((128, 8, 512), np.float32)
    p = np.arange(128)[:, None]
    f = np.arange(512)[None, :]
    for jj in range(8):
        kb = 7 - jj
        kpos = kb * 128 + p
        qpos = half * 512 + f
        m[:, jj, :] = np.where(kpos < qpos, 0.0, NEG)
    return m.reshape(128, 8 * 512)


def colT(v, n):
    return np.ascontiguousarray(np.asarray(v, np.float32).reshape(n, 128).T)


def make_in_maps(inp):
    ident, ntri, blk, sel = make_consts()
    maps = []
    x = np.asarray(inp["x"], np.float32)
    f = lambda k: np.asarray(inp[k], np.float32)[0]
    for core in range(NCORES):
        b, half = core // 2, core % 2
        tiles = [2 * k + half for k in range(4)]
        x_own = np.concatenate([x[b, t * 512:(t + 1) * 512] for t in tiles], axis=0)
        x_halo = np.zeros((64, D), np.float32)
        halo_mask = np.zeros((128, 64), np.float32)
        invcnt = np.zeros((128, 4, 16), np.float32)
        for k, t in enumerate(tiles):
            if t > 0:
                x_halo[k * 16:(k + 1) * 16] = x[b, t * 512 - 16:t * 512]
                halo_mask[:, k * 16:(k + 1) * 16] = 1.0
        for g, w in enumerate((2, 4, 8, 16)):
            if tiles[0] == 0:
                invcnt[:, g, :] = 1.0 / np.minimum(np.arange(16) + 1, w)[None, :]
            else:
                invcnt[:, g, :] = 1.0 / w
        m = {
            "x_all": np.ascontiguousarray(x[b]),
            "x_own": np.ascontiguousarray(x_own),
            "x_halo": x_halo,
            "halo_mask": halo_mask,
            "invcnt": invcnt.reshape(128, 64),
            "maskb": make_maskb(half),
            "c_in": colT(np.asarray(inp["c"], np.float32)[b], 8),
            "w_ada": f("w_ada"),
            "b_ada": f("b_ada").reshape(1, -1),
            "b_adaT": colT(f("b_ada"), 48),
            "g_norm1T": colT(f("g_norm1"), 8),
            "w_in": f("w_in"),
            "g_q2": np.ascontiguousarray(np.tile(f("g_q"), 2).reshape(128, 1)),
            "g_k2": np.ascontiguousarray(np.tile(f("g_k"), 2).reshape(128, 1)),
            "w_sb_out": f("w_sb_out"),
            "w_pool_group": f("w_pool_group"),
            "pool_scaleT": colT(f("pool_scale"), 4),
            "w_pool_out": f("w_pool_out"),
            "w_out": f("w_out"),
            "g_norm2T": colT(f("g_norm2"), 8),
            "w_router": f("w_router"),
            "router_bias": f("router_bias").reshape(1, -1),
            "w_exp_gate": f("w_exp_gate"),
            "w_exp_up": f("w_exp_up"),
            "w_exp_down": f("w_exp_down"),
            "w_sh_gate": f("w_sh_gate"),
            "w_sh_up": f("w_sh_up"),
            "w_sh_down": f("w_sh_down"),
            "c_ident": ident, "c_ntri": ntri, "c_blk": blk, "c_sel": sel,
        }
        maps.append(m)
    return maps


def kernel(**inputs):
    nc, _ = build()
    in_maps = make_in_maps(inputs)
    res = run_bass_kernel_spmd(nc, in_maps, core_ids=list(range(NCORES)))
    outp = np.zeros((4, S, D), np.float32)
    for core in range(NCORES):
        b, half = core // 2, core % 2
        o = np.asarray(res.results[core]["out"]).reshape(TOK_OWN, D)
        for k in range(4):
            t = 2 * k + half
            outp[b, t * 512:(t + 1) * 512] = o[k * 512:(k + 1) * 512]
    return outp
```
